# Optimizing a Trainium2 kernel written in Bass

```python
import math
import jax, jax.numpy as jnp
from jax import lax
import numpy as np

D_MODEL = 1024
BATCH = 8
SEQ = 4096
DEPTH = 2

HEAD_DIM = 64
A_HEADS_PER_GROUP = 16
DIL_PAIRS = ((128, 1), (512, 4), (2048, 16))
N_DIL_GROUPS = len(DIL_PAIRS)
DIL_BLOCK = 128
B_HEADS = D_MODEL // HEAD_DIM
Q_BLOCK = 128
NUM_BUCKETS = 32
MAX_DISTANCE = 2048
N_EXPERTS = 32
TOP_K = 4
D_FF = D_MODEL
SWIGLU_LIMIT = 7.0
SWIGLU_ALPHA = 1.702
MOE_BLOCK = 256
RMS_EPS = 1e-6
NEG = -1e30

kernel_name = 'yoco_dilated_fox_moe_block'


def rms_norm(x, g):
    xf = x.astype(jnp.float32)
    y = xf * lax.rsqrt(jnp.mean(xf * xf, axis=-1, keepdims=True) + RMS_EPS)
    return (y * g.astype(jnp.float32)).astype(x.dtype)


def modulate(h, shift, scale):
    return h * (1.0 + scale[:, None, :]) + shift[:, None, :]


def t5_bucket(n):
    max_exact = NUM_BUCKETS // 2
    nf = jnp.maximum(n, 1).astype(jnp.float32)
    large = max_exact + (jnp.log(nf / max_exact) / math.log(MAX_DISTANCE / max_exact)
                         * (NUM_BUCKETS - max_exact)).astype(jnp.int32)
    large = jnp.minimum(large, NUM_BUCKETS - 1)
    return jnp.where(n < max_exact, n, large)


def dilated_group_attention(q, k, v, bias_tab, dilation, window):
    Bq, S, H, dh = q.shape
    steps = window // dilation
    L = -(-S // dilation)
    nb = -(-L // DIL_BLOCK)
    Lp = nb * DIL_BLOCK
    Sp = Lp * dilation

    def to_blocks(t):
        t = jnp.pad(t, ((0, 0), (0, Sp - S), (0, 0), (0, 0)))
        t = t.reshape(Bq, Lp, dilation, H, dh).transpose(0, 2, 3, 1, 4)
        return t.reshape(Bq, dilation, H, nb, DIL_BLOCK, dh)

    def with_prev(t):
        prev = jnp.pad(t[:, :, :, :-1], ((0, 0), (0, 0), (0, 0), (1, 0), (0, 0), (0, 0)))
        return jnp.concatenate([prev, t], axis=4)

    qb = to_blocks(q)
    kk = with_prev(to_blocks(k))
    vv = with_prev(to_blocks(v))

    qi = jnp.arange(DIL_BLOCK, dtype=jnp.int32)[:, None]
    kj = jnp.arange(2 * DIL_BLOCK, dtype=jnp.int32)[None, :]
    delta = qi + DIL_BLOCK - kj
    blk = jnp.arange(nb, dtype=jnp.int32)[:, None, None]
    valid = (delta >= 0) & (delta <= steps) & (blk * DIL_BLOCK + kj - DIL_BLOCK >= 0)
    bucket = t5_bucket(jnp.clip(delta, 0, None) * dilation)
    bias = bias_tab[bucket].astype(jnp.float32).transpose(2, 0, 1)

    s = jnp.einsum('brhnqd,brhnkd->brhnqk', qb, kk).astype(jnp.float32) * (HEAD_DIM ** -0.5)
    s = jnp.where(valid, s + bias[:, None], NEG)
    m = jnp.max(s, axis=-1, keepdims=True)
    p = jnp.exp(s - m)
    den = jnp.sum(p, axis=-1, keepdims=True)
    o = jnp.einsum('brhnqk,brhnkd->brhnqd', (p / den).astype(v.dtype), vv)
    lse = (m + jnp.log(den))[..., 0]

    o = o.reshape(Bq, dilation, H, Lp, dh).transpose(0, 3, 1, 2, 4).reshape(Bq, Sp, H, dh)[:, :S]
    lse = lse.reshape(Bq, dilation, H, Lp).transpose(0, 3, 1, 2).reshape(Bq, Sp, H)[:, :S]
    return o, lse


def dilated_mixer(h, w_qkv, w_o, rel_bias):
    Bq, S, _ = h.shape
    qkv = (h @ w_qkv).reshape(Bq, S, N_DIL_GROUPS, 3, A_HEADS_PER_GROUP, HEAD_DIM)
    outs, lses = [], []
    for g, (window, dilation) in enumerate(DIL_PAIRS):
        tab = rel_bias[:, g * A_HEADS_PER_GROUP:(g + 1) * A_HEADS_PER_GROUP]
        o, lse = dilated_group_attention(qkv[:, :, g, 0], qkv[:, :, g, 1], qkv[:, :, g, 2],
                                         tab, dilation, window)
        outs.append(o)
        lses.append(lse)
    wts = jax.nn.softmax(jnp.stack(lses, axis=0), axis=0)
    o = jnp.sum(wts[..., None] * jnp.stack(outs, axis=0).astype(jnp.float32), axis=0)
    o = o.astype(h.dtype).reshape(Bq, S, A_HEADS_PER_GROUP * HEAD_DIM)
    return o @ w_o


def shared_kv(x, kv_norm_g, w_kvf, b_f):
    Bq, S, _ = x.shape
    z = rms_norm(x, kv_norm_g) @ w_kvf
    nkv = B_HEADS * HEAD_DIM
    k = z[..., :nkv].reshape(Bq, S, B_HEADS, HEAD_DIM)
    v = z[..., nkv:2 * nkv].reshape(Bq, S, B_HEADS, HEAD_DIM)
    log_f = jax.nn.log_sigmoid(z[..., 2 * nkv:].astype(jnp.float32) + b_f.astype(jnp.float32))
    fcum = jnp.cumsum(log_f, axis=1)
    return k, v, fcum


def forgetting_attention(q, k, v, fcum):
    Bq, S, H, dh = q.shape
    nb = S // Q_BLOCK
    qb = q.reshape(Bq, nb, Q_BLOCK, H, dh).transpose(1, 0, 3, 2, 4)
    fq = fcum.reshape(Bq, nb, Q_BLOCK, H).transpose(1, 0, 3, 2)
    kh = k.transpose(0, 2, 1, 3)
    vh = v.transpose(0, 2, 1, 3)
    fk = fcum.transpose(0, 2, 1)
    kpos = jnp.arange(S, dtype=jnp.int32)

    def block(args):
        q_i, f_i, i = args
        s = jnp.einsum('bhqd,bhkd->bhqk', q_i, kh).astype(jnp.float32) * (dh ** -0.5)
        s = s + f_i[..., None] - fk[:, :, None, :]
        qpos = i * Q_BLOCK + jnp.arange(Q_BLOCK, dtype=jnp.int32)
        s = jnp.where(kpos[None, :] <= qpos[:, None], s, NEG)
        p = jax.nn.softmax(s, axis=-1)
        return jnp.einsum('bhqk,bhkd->bhqd', p.astype(vh.dtype), vh)

    o = lax.map(block, (qb, fq, jnp.arange(nb, dtype=jnp.int32)))
    return o.transpose(1, 0, 3, 2, 4).reshape(Bq, S, H, dh)


def forgetting_mixer(h, w_q, w_o, k, v, fcum):
    Bq, S, _ = h.shape
    q = (h @ w_q).reshape(Bq, S, B_HEADS, HEAD_DIM)
    o = forgetting_attention(q, k, v, fcum).reshape(Bq, S, B_HEADS * HEAD_DIM)
    return o @ w_o


def moe_ffn(h, w_router, b_router, w_gu, b_gu, w_down, b_down):
    Bq, S, D = h.shape
    T = Bq * S
    n_slots = T * TOP_K
    xf = h.reshape(T, D)
    logits = (xf @ w_router).astype(jnp.float32) + b_router.astype(jnp.float32)
    top_val, top_idx = lax.top_k(logits, TOP_K)
    gates = jax.nn.softmax(top_val, axis=-1)

    e_flat = top_idx.reshape(-1).astype(jnp.int32)
    tok_flat = jnp.arange(n_slots, dtype=jnp.int32) // TOP_K
    order = jnp.argsort(e_flat)
    e_sorted = e_flat[order]
    tok_sorted = tok_flat[order]
    gate_sorted = gates.reshape(-1)[order]

    counts = jnp.bincount(e_flat, length=N_EXPERTS)
    starts = jnp.cumsum(counts) - counts
    padded = (counts + MOE_BLOCK - 1) // MOE_BLOCK * MOE_BLOCK
    pad_ends = jnp.cumsum(padded)
    pad_starts = pad_ends - padded
    dest = pad_starts[e_sorted] + (jnp.arange(n_slots, dtype=jnp.int32) - starts[e_sorted])

    n_rows = n_slots + N_EXPERTS * MOE_BLOCK
    n_blocks = n_rows // MOE_BLOCK
    xbuf = jnp.zeros((n_rows, D), h.dtype).at[dest].set(xf[tok_sorted])
    block_start = jnp.arange(n_blocks, dtype=jnp.int32) * MOE_BLOCK
    block_expert = jnp.minimum(jnp.searchsorted(pad_ends, block_start, side='right'), N_EXPERTS - 1)

    def expert_block(args):
        xb, e = args
        gu = xb @ w_gu[e] + b_gu[e]
        g, u = jnp.split(gu, 2, axis=-1)
        g = jnp.minimum(g, SWIGLU_LIMIT)
        u = jnp.clip(u, -SWIGLU_LIMIT, SWIGLU_LIMIT)
        act = (u + 1.0) * g * jax.nn.sigmoid(SWIGLU_ALPHA * g)
        return act @ w_down[e] + b_down[e]

    ybuf = lax.map(expert_block, (xbuf.reshape(n_blocks, MOE_BLOCK, D), block_expert))
    y_slots = ybuf.reshape(n_rows, D)[dest] * gate_sorted[:, None].astype(h.dtype)
    out = jnp.zeros((T, D), h.dtype).at[tok_sorted].add(y_slots)
    return out.reshape(Bq, S, D)


def setup_inputs(seed: int = 0) -> dict:
    key = jax.random.key(seed)
    ks = jax.random.split(key, 24)
    D = D_MODEL
    n_a = DEPTH // 2
    n_b = DEPTH - n_a

    def nrm(k, shape, scale):
        return jax.random.normal(k, shape, jnp.float32) * scale

    a_width = A_HEADS_PER_GROUP * HEAD_DIM
    b_width = B_HEADS * HEAD_DIM
    return {
        'x': nrm(ks[0], (BATCH, SEQ, D), 1.0),
        'c': nrm(ks[1], (BATCH, D), 1.0),
        'ada_w': nrm(ks[2], (DEPTH, D, 6 * D), 0.5 * D ** -0.5),
        'ada_b': nrm(ks[3], (DEPTH, 6 * D), 0.02),
        'norm_mix_g': 1.0 + nrm(ks[4], (DEPTH, D), 0.02),
        'norm_ffn_g': 1.0 + nrm(ks[5], (DEPTH, D), 0.02),
        'a_w_qkv': nrm(ks[6], (n_a, D, N_DIL_GROUPS * 3 * a_width), D ** -0.5),
        'a_w_o': nrm(ks[7], (n_a, a_width, D), a_width ** -0.5),
        'rel_bias': nrm(ks[8], (NUM_BUCKETS, N_DIL_GROUPS * A_HEADS_PER_GROUP), 0.5),
        'kv_norm_g': 1.0 + nrm(ks[9], (D,), 0.02),
        'w_kvf': nrm(ks[10], (D, 2 * b_width + B_HEADS), D ** -0.5),
        'b_f': jax.random.uniform(ks[11], (B_HEADS,), jnp.float32, 1.0, 5.0),
        'b_w_q': nrm(ks[12], (n_b, D, b_width), D ** -0.5),
        'b_w_o': nrm(ks[13], (n_b, b_width, D), b_width ** -0.5),
        'router_w': nrm(ks[14], (DEPTH, D, N_EXPERTS), D ** -0.5),
        'router_b': nrm(ks[15], (DEPTH, N_EXPERTS), 0.01),
        'w_gu': nrm(ks[16], (DEPTH, N_EXPERTS, D, 2 * D_FF), D ** -0.5),
        'b_gu': nrm(ks[17], (DEPTH, N_EXPERTS, 2 * D_FF), 0.01),
        'w_down': nrm(ks[18], (DEPTH, N_EXPERTS, D_FF, D), D_FF ** -0.5),
        'b_down': nrm(ks[19], (DEPTH, N_EXPERTS, D), 0.01),
        'final_norm_g': 1.0 + nrm(ks[20], (D,), 0.02),
    }


def reference(x, c, ada_w, ada_b, norm_mix_g, norm_ffn_g, a_w_qkv, a_w_o, rel_bias,
              kv_norm_g, w_kvf, b_f, b_w_q, b_w_o, router_w, router_b, w_gu, b_gu,
              w_down, b_down, final_norm_g):
    n_a = DEPTH // 2
    c_act = jax.nn.silu(c)
    k_sh = v_sh = f_sh = None
    for layer in range(DEPTH):
        ada = c_act @ ada_w[layer] + ada_b[layer]
        shift_m, scale_m, gate_m, shift_f, scale_f, gate_f = jnp.split(ada, 6, axis=-1)
        h = modulate(rms_norm(x, norm_mix_g[layer]), shift_m, scale_m)
        if layer < n_a:
            mix = dilated_mixer(h, a_w_qkv[layer], a_w_o[layer], rel_bias)
        else:
            if layer == n_a:
                k_sh, v_sh, f_sh = shared_kv(x, kv_norm_g, w_kvf, b_f)
            mix = forgetting_mixer(h, b_w_q[layer - n_a], b_w_o[layer - n_a], k_sh, v_sh, f_sh)
        x = x + gate_m[:, None, :] * mix
        h = modulate(rms_norm(x, norm_ffn_g[layer]), shift_f, scale_f)
        x = x + gate_f[:, None, :] * moe_ffn(h, router_w[layer], router_b[layer], w_gu[layer],
                                             b_gu[layer], w_down[layer], b_down[layer])
    return rms_norm(x, final_norm_g)
```

```python
import math
import os
from contextlib import ExitStack

import ml_dtypes
import numpy as np

import concourse.bass as bass
import concourse.mybir as mybir
from concourse.bass_utils import run_bass_kernel_spmd

F32 = mybir.dt.float32
BF16 = mybir.dt.bfloat16
AF = mybir.ActivationFunctionType
ALU = mybir.AluOpType
AX = mybir.AxisListType

S = 4096
D = 1024
NT = S // 128
NE = 32
DIL = ((128, 1), (512, 4), (2048, 16))
NEG = -1e30
BLK = 512
NBLK = (4 * S + NE * (BLK - 1)) // BLK
NROWS = NBLK * BLK
I32 = mybir.dt.int32
DEBUG = False


class Eng:
    def __init__(self, name, sem, h):
        self.name, self.sem, self.h = name, sem, h
        self.count = 0
        self.seen = {}


class Buf:
    def __init__(self, name):
        self.name = name
        self.w = None
        self.r = {}


class Tile:
    def __init__(self, t, name):
        self.t = t
        self.b = Buf(name)

    def __getitem__(self, k):
        return self.t[k]


class Tr:
    def __init__(self, nc, es):
        self.nc, self.es = nc, es
        self.E = {}
        for n, h in (("pe", nc.tensor), ("act", nc.scalar), ("dve", nc.vector),
                     ("pool", nc.gpsimd), ("sp", nc.sync)):
            self.E[n] = Eng(n, es.enter_context(nc.semaphore("s_" + n)), h)
        self.slots = []
        self.nslot = 0
        for i in range(48):
            self.slots.append(Eng("q%d" % i, es.enter_context(nc.semaphore("q%d" % i)), None))

        self.swslots = []
        self.nsw = 0
        for i in range(40):
            self.swslots.append(Eng("w%d" % i, es.enter_context(nc.semaphore("w%d" % i)), None))

    def slot(self):
        e = self.slots[self.nslot]
        self.nslot += 1
        return e

    def swslot(self):
        e = self.swslots[self.nsw]
        self.nsw += 1
        return e

    def reset_slots(self):
        self.nslot = 0
        self.nsw = 0

    def _deps(self, e, reads, writes, same):
        deps = {}

        def add(eng, cnt):
            if deps.get(eng, 0) < cnt:
                deps[eng] = cnt

        for b in reads:
            if b.w is not None:
                add(*b.w)
        for b in writes:
            if b.w is not None:
                add(*b.w)
            for eng, cnt in b.r.items():
                add(eng, cnt)
        for eng, cnt in deps.items():
            if eng is e and not same:
                continue
            if e.seen.get(eng, 0) < cnt:
                e.h.wait_ge(eng.sem, cnt)
                e.seen[eng] = cnt

    def op(self, en, fn, R=(), W=(), same=True):
        e = self.E[en]
        R = [x.b if isinstance(x, Tile) else x for x in R]
        W = [x.b if isinstance(x, Tile) else x for x in W]
        self._deps(e, R, W, same and en != "pe")
        e.count += 1
        fn(e.h).then_inc(e.sem, 1)
        for b in R:
            b.r[e] = e.count
        for b in W:
            b.w = (e, e.count)
            b.r = {}

    def dma(self, en, slot, out, in_, R=(), W=(), **kw):
        e = self.E[en]
        R = [x.b if isinstance(x, Tile) else x for x in R]
        W = [x.b if isinstance(x, Tile) else x for x in W]
        self._deps(e, R, W, True)
        if e.seen.get(slot, 0) < slot.count:
            e.h.wait_ge(slot.sem, slot.count)
            e.seen[slot] = slot.count
        slot.count += 16
        e.h.dma_start(out=out, in_=in_, **kw).then_inc(slot.sem, 16)
        for b in R:
            b.r[slot] = slot.count
        for b in W:
            b.w = (slot, slot.count)
            b.r = {}

    def idma(self, slot, out, out_off, in_, in_off, bound, R=(), W=()):
        e = self.E["pool"]
        R = [x.b if isinstance(x, Tile) else x for x in R]
        W = [x.b if isinstance(x, Tile) else x for x in W]
        self._deps(e, R, W, True)
        if e.seen.get(slot, 0) < slot.count:
            e.h.wait_ge(slot.sem, slot.count)
            e.seen[slot] = slot.count
        slot.count += 16
        e.h.indirect_dma_start(out=out, out_offset=out_off, in_=in_, in_offset=in_off,
                               bounds_check=self.bound_reg, oob_is_err=False).then_inc(slot.sem, 16)
        for b in R:
            b.r[slot] = slot.count
        for b in W:
            b.w = (slot, slot.count)
            b.r = {}

    def wait_slots(self, en, slots):
        e = self.E[en]
        for o in slots:
            if e.seen.get(o, 0) < o.count:
                e.h.wait_ge(o.sem, o.count)
                e.seen[o] = o.count

    def barrier(self):
        allq = list(self.E.values()) + self.slots + self.swslots
        for e in self.E.values():
            for o in allq:
                if o is e or o.count == 0:
                    continue
                if e.seen.get(o, 0) < o.count:
                    e.h.wait_ge(o.sem, o.count)
                    e.seen[o] = o.count
        self.reset_slots()

    def mm(self, out, lhsT, rhs, start=True, stop=True, R=(), W=()):
        self.op("pe", lambda h: h.matmul(out, lhsT, rhs, start=start, stop=stop), R, W)

    def tr(self, out, in_, ident, R=(), W=()):
        self.op("pe", lambda h: h.transpose(out, in_, ident), R, W)

    def act(self, out, in_, func, bias=0.0, scale=1.0, accum=None, R=(), W=(), same=True):
        if accum is None:
            self.op("act", lambda h: h.activation(out, in_, func, bias=bias, scale=scale), R, W, same)
        else:
            self.op("act", lambda h: h.activation(out, in_, func, bias=bias, scale=scale, accum_out=accum), R, W)

    def tt(self, en, out, a, b, op, R=(), W=(), same=True):
        self.op(en, lambda h: h.tensor_tensor(out, a, b, op), R, W, same)

    def ts(self, en, out, a, s1, s2, op0, op1=None, R=(), W=()):
        if op1 is None:
            self.op(en, lambda h: h.tensor_scalar(out, a, s1, None, op0), R, W)
        else:
            self.op(en, lambda h: h.tensor_scalar(out, a, s1, s2, op0, op1), R, W)

    def stt(self, out, in0, scalar, in1, op0, op1, accum=None, R=(), W=()):
        if accum is None:
            self.op("dve", lambda h: h.scalar_tensor_tensor(out, in0, scalar, in1, op0, op1), R, W)
        else:
            self.op("dve", lambda h: h.scalar_tensor_tensor(out, in0, scalar, in1, op0, op1, accum_out=accum), R, W)

    def cp(self, en, out, in_, R=(), W=()):
        if en == "act":
            self.op("act", lambda h: h.copy(out, in_), R, W)
        else:
            self.op(en, lambda h: h.tensor_copy(out, in_), R, W)

    def memset(self, en, ap, val, W=()):
        self.op(en, lambda h: h.memset(ap, val), (), W)

    def recip(self, out, in_, R=(), W=()):
        self.op("dve", lambda h: h.reciprocal(out, in_), R, W)


class K:
    pass


def sb(k, es, name, shape, dt):
    k.uid += 1
    nm = "%s_%d" % (name, k.uid)
    return Tile(es.enter_context(k.nc.sbuf_tensor(nm, shape, dt)), nm)


def ps(k, es, name, shape, dt):
    k.uid += 1
    nm = "%s_%d" % (name, k.uid)
    return Tile(es.enter_context(k.nc.psum_tensor(nm, shape, dt)), nm)


def bcast_row(ap_row, n=128):
    return ap_row.partition_broadcast(n)


def phase_ada(k):
    nc, T = k.nc, k.T
    with ExitStack() as es:
        ccol = sb(k, es, "ccol", [128, 8], F32)
        w = [sb(k, es, "adaw", [128, 8, 512], F32) for _ in range(2)]
        wq = [T.slot() for _ in range(2)]
        arow = sb(k, es, "arow", [1, 12, 1024], F32)
        brow = sb(k, es, "brow", [1, 12, 1024], F32)
        grow = sb(k, es, "grow", [1, 4, 1024], F32)
        pst = [ps(k, es, "adaps", [1, 512], F32) for _ in range(2)]
        q0 = T.slot()
        T.dma("sp", q0, ccol[:], k.d["c_col"], W=[ccol])
        T.dma("sp", q0, brow[:], k.d["ada_b"].rearrange("(o l) (j f) -> o (l j) f", o=1, f=1024), W=[brow])
        T.dma("sp", q0, grow[:, 0:2, :], k.d["norm_mix_g"].rearrange("(o l) f -> o l f", o=1), W=[grow])
        T.dma("sp", q0, grow[:, 2:4, :], k.d["norm_ffn_g"].rearrange("(o l) f -> o l f", o=1), W=[grow])
        T.act(ccol[:], ccol[:], AF.Silu, R=[ccol], W=[ccol])
        it = 0
        for l in range(2):
            wv = k.d["ada_w"][l].rearrange("(kk p) n -> p kk n", p=128)
            for n in range(12):
                wb = w[it % 2]
                T.dma("sp", wq[it % 2], wb[:], wv[:, :, n * 512:(n + 1) * 512], W=[wb])
                pt = pst[it % 2]
                for kk in range(8):
                    T.mm(pt[:], ccol[:, kk:kk + 1], wb[:, kk, :], start=(kk == 0), stop=(kk == 7),
                         R=[ccol, wb], W=[pt])
                j, half = divmod(n, 2)
                r = l * 6 + j
                T.tt("dve", arow[:, r, half * 512:(half + 1) * 512], pt[:],
                     brow[:, r, half * 512:(half + 1) * 512], ALU.add, R=[pt, brow], W=[arow])
                it += 1
        for l in range(2):
            T.stt(arow[:, l * 6 + 1, :], arow[:, l * 6 + 1, :], 1.0, grow[:, l, :], ALU.add, ALU.mult,
                  R=[arow, grow], W=[arow])
            T.stt(arow[:, l * 6 + 4, :], arow[:, l * 6 + 4, :], 1.0, grow[:, 2 + l, :], ALU.add, ALU.mult,
                  R=[arow, grow], W=[arow])
        T.dma("sp", q0, k.d["ada_scr"].rearrange("(o r) f -> o r f", o=1), arow[:], R=[arow], W=[k.b_ada])
    T.barrier()


ROW_SHIFT_M, ROW_GS_M, ROW_GATE_M, ROW_SHIFT_F, ROW_GS_F, ROW_GATE_F = range(6)


def ada_row(k, l, which):
    r = l * 6 + which
    return k.d["ada_scr"][r:r + 1, :]


def rstd_a(k, xt, junk, ss):
    T = k.T
    T.stt(junk[:], xt[:], 1.0, xt[:], ALU.mult, ALU.mult, accum=ss[:, 0:1], R=[xt], W=[junk, ss])
    T.ts("dve", ss[:, 1:2], ss[:, 0:1], 1.0 / D, 1e-6, ALU.mult, ALU.add, R=[ss], W=[ss])
    T.act(ss[:, 2:3], ss[:, 1:2], AF.Sqrt, R=[ss], W=[ss])


def rstd_b(k, ss, rstd):
    k.T.recip(rstd[:, 0:1], ss[:, 2:3], R=[ss], W=[rstd])


def rstd_of(k, xt, junk, ss, rstd):
    rstd_a(k, xt, junk, ss)
    rstd_b(k, ss, rstd)


def phase_prenorm(k, es_out, xs, layer, with_kv=False):
    nc, T = k.nc, k.T
    x_src, b_src = k.d[xs], k.db[xs]
    hT = sb(k, es_out, "hT", [128, 8, S], BF16)
    hkT = sb(k, es_out, "hkT", [128, 8, S], BF16) if with_kv else None
    with ExitStack() as es:
        gs = sb(k, es, "gs", [128, D], F32)
        sh = sb(k, es, "sh", [128, D], F32)
        kg = sb(k, es, "kg", [128, D], F32) if with_kv else None
        q0 = T.slot()
        T.dma("sp", q0, gs[:], bcast_row(ada_row(k, layer, ROW_GS_M)), R=[k.b_ada], W=[gs])
        T.dma("sp", q0, sh[:], bcast_row(ada_row(k, layer, ROW_SHIFT_M)), R=[k.b_ada], W=[sh])
        if with_kv:
            T.dma("sp", q0, kg[:], bcast_row(k.d["kv_norm_g"].rearrange("(o f) -> o f", o=1)), W=[kg])
        xt = [sb(k, es, "xt", [128, D], F32) for _ in range(2)]
        xq = [T.slot() for _ in range(2)]
        junk = sb(k, es, "junk", [128, D], F32)
        hf = sb(k, es, "hf", [128, D], F32)
        hb = [sb(k, es, "hb", [128, D], BF16) for _ in range(2)]
        hkb = [sb(k, es, "hkb", [128, D], BF16) for _ in range(2)] if with_kv else None
        ss = [sb(k, es, "ss", [128, 4], F32) for _ in range(2)]
        rstd = [sb(k, es, "rstd", [128, 1], F32) for _ in range(2)]
        pst = [ps(k, es, "pst", [128, D], BF16) for _ in range(2)]
        pst2 = [ps(k, es, "pst2", [128, D], BF16) for _ in range(2)] if with_kv else None
        ident = k.ident_bf
        def s1(i):
            p = i % 2
            T.dma("sp", xq[p], xt[p][:], x_src[i * 128:(i + 1) * 128, :], R=[b_src], W=[xt[p]])
            rstd_a(k, xt[p], junk, ss[p])

        def s2(i):
            p = i % 2
            rstd_b(k, ss[p], rstd[p])
            T.stt(hf[:], xt[p][:], rstd[p][:, 0:1], gs[:], ALU.mult, ALU.mult, R=[xt[p], rstd[p], gs], W=[hf])
            T.tt("pool", hb[p][:], hf[:], sh[:], ALU.add, R=[hf, sh], W=[hb[p]])
            for j in range(8):
                T.tr(pst[p][:, j * 128:(j + 1) * 128], hb[p][:, j * 128:(j + 1) * 128], ident[:],
                     R=[hb[p], ident], W=[pst[p]])
            T.cp("act", hT[:, :, i * 128:(i + 1) * 128], pst[p][:].rearrange("p (j t) -> p j t", j=8),
                 R=[pst[p]], W=[hT])
            if with_kv:
                T.stt(hkb[p][:], xt[p][:], rstd[p][:, 0:1], kg[:], ALU.mult, ALU.mult,
                      R=[xt[p], rstd[p], kg], W=[hkb[p]])
                for j in range(8):
                    T.tr(pst2[p][:, j * 128:(j + 1) * 128], hkb[p][:, j * 128:(j + 1) * 128], ident[:],
                         R=[hkb[p], ident], W=[pst2[p]])
                T.cp("act", hkT[:, :, i * 128:(i + 1) * 128], pst2[p][:].rearrange("p (j t) -> p j t", j=8),
                     R=[pst2[p]], W=[hkT])

        s1(0)
        for i in range(NT):
            if i + 1 < NT:
                s1(i + 1)
            s2(i)
    T.barrier()
    return hT, hkT


def tok_slice(d, blk):
    nb = 32 // d
    r, b = divmod(blk, nb)
    start = d * 128 * b + r
    return slice(start, start + d * 127 + 1, d), b


def phase_dilated(k, hT):
    nc, T = k.nc, k.T
    with ExitStack() as es:
        wqkv = [[sb(k, es, "wqkv", [128, 8, 128], BF16) for _ in range(3)] for _ in range(2)]
        wslot = [T.swslot() for _ in range(2)]
        QT = sb(k, es, "QT", [128, S], BF16)
        KT = sb(k, es, "KT", [128, S], BF16)
        VA0 = sb(k, es, "VA0", [128, 32, 128], BF16)
        V0B = sb(k, es, "V0B", [128, 32, 128], BF16)
        bm = [sb(k, es, "bm", [128, 512], F32) for _ in range(2)]
        bslot = [T.slot() for _ in range(2)]
        acc = sb(k, es, "acc", [128, 2, S], F32)
        obf = sb(k, es, "obf", [128, S], BF16)
        oslot = T.slot()
        sf = [sb(k, es, "sf", [128, 512], F32) for _ in range(2)]
        pT = [sb(k, es, "pT", [128, 512], BF16) for _ in range(2)]
        pqk = [ps(k, es, "pqk", [128, 512], F32) for _ in range(2)]
        pvt = pqk[1]
        pss2 = [ps(k, es, "pss", [128, 2, 512], F32) for _ in range(2)]
        pod = [ps(k, es, "pod", [128, 2, 128], F32) for _ in range(2)]
        T.memset("pool", VA0[:], 0.0, W=[VA0])
        T.memset("pool", V0B[:], 0.0, W=[V0B])
        wview = k.d["a_w_qkv"].rearrange("(kk p) n -> p kk n", p=128)
        order = [(hp, g) for hp in range(8) for g in range(3)]
        import os
        order = order[:int(os.environ.get("DIL_N", "24"))]
        dstop = int(os.environ.get("DIL_STOP", "9"))

        def load_w(idx):
            hp, g = order[idx]
            p = idx % 2
            for j in range(3):
                c0 = g * 3072 + j * 1024 + hp * 128
                T.dma("pool", wslot[p], wqkv[p][j][:], wview[:, :, c0:c0 + 128], W=[wqkv[p][j]])
            T.dma("sp", bslot[p], bm[p][:], k.d["dil_bias"][g, hp], W=[bm[p]])
            T.tt("pool", bm[p][:], bm[p][:], k.dmask[:], ALU.add, R=[bm[p], k.dmask], W=[bm[p]])

        load_w(0)
        it = 0
        for idx, (hp, g) in enumerate(order):
            p = idx % 2
            if idx + 1 < len(order):
                load_w(idx + 1)
            wq_, wk_, wv_ = wqkv[p]
            d = DIL[g][1]
            if dstop < 1:
                continue
            for dst, wt in (() if os.environ.get('DIL_SKIPQK') else ((QT, wq_), (KT, wk_))):
                for tt_ in range(8):
                    pp = pqk[it % 2]
                    it += 1
                    for kk in range(8):
                        T.mm(pp[:], wt[:, kk, :], hT[:, kk, tt_ * 512:(tt_ + 1) * 512],
                             start=(kk == 0), stop=(kk == 7), R=[wt, hT], W=[pp])
                    T.cp("act", dst[:, tt_ * 512:(tt_ + 1) * 512], pp[:], R=[pp], W=[dst])
            if dstop < 2:
                continue
            for b4 in range(8):
                for bi in range(4):
                    blk = b4 * 4 + bi
                    sl, _ = tok_slice(d, blk)
                    for kk in range(8):
                        T.mm(pvt[:, bi * 128:(bi + 1) * 128], hT[:, kk, sl], wv_[:, kk, :], start=(kk == 0), stop=(kk == 7),
                             R=[hT, wv_], W=[pvt])
                pv3 = pvt[:].rearrange("p (a c) -> p a c", a=4)
                T.cp("act", VA0[:, b4 * 4:(b4 + 1) * 4, 0:64], pv3[:, :, 0:64], R=[pvt], W=[VA0])
                T.cp("dve", V0B[:, b4 * 4:(b4 + 1) * 4, 64:128], pv3[:, :, 64:128], R=[pvt, VA0], W=[V0B])
            if dstop < 3:
                continue
            def s_part(blk):
                sl, b = tok_slice(d, blk)
                slp, _ = tok_slice(d, blk - 1) if b > 0 else (None, None)
                c0 = 0
                pss = pss2[blk % 2]
                s_b = pss
                for hh in range(2):
                    pb = slice(hh * 64, hh * 64 + 64)
                    if b > 0:
                        T.mm(pss[:, hh, c0:c0 + 128], KT[pb, slp], QT[pb, sl], R=[KT, QT], W=[s_b])
                    T.mm(pss[:, hh, c0 + 128:c0 + 256], KT[pb, sl], QT[pb, sl], R=[KT, QT], W=[s_b])
                sfb, pTb = sf[blk % 2], pT[blk % 2]
                v3 = lambda t: t[:].rearrange("p (h cq) -> p h cq", h=2)
                if b > 0:
                    T.stt(v3(sfb), pss[:, :, c0:c0 + 256], 0.125, v3(bm[p]), ALU.mult, ALU.add,
                          R=[s_b, bm[p]], W=[sfb])
                    T.act(pTb[:], sfb[:], AF.Exp, R=[sfb], W=[pTb])
                else:
                    T.stt(v3(sfb)[:, :, 128:256], pss[:, :, c0 + 128:c0 + 256], 0.125, v3(bm[p])[:, :, 128:256],
                          ALU.mult, ALU.add, R=[s_b, bm[p]], W=[sfb])
                    T.act(v3(pTb)[:, :, 128:256], v3(sfb)[:, :, 128:256], AF.Exp, R=[sfb], W=[pTb])

            def pv_part(blk):
                sl, b = tok_slice(d, blk)
                pTb = pT[blk % 2]
                od = pod[blk % 2]
                for which in range(2):
                    mms = []
                    for hh in range(2):
                        for kc in range(2):
                            if kc == 0 and b == 0:
                                continue
                            vb = blk - 1 if kc == 0 else blk
                            if which == 0:
                                lhs = (VA0 if hh == 0 else V0B)[:, vb, :]
                            else:
                                lhs = (k.onesA0 if hh == 0 else k.ones0B)[:]
                            mms.append((lhs, pTb[:, (hh * 2 + kc) * 128:(hh * 2 + kc + 1) * 128]))
                    for mi, (lhs, rhs) in enumerate(mms):
                        T.mm(od[:, which, :], lhs, rhs, start=(mi == 0), stop=(mi == len(mms) - 1),
                             R=[VA0, V0B, pTb, k.onesA0, k.ones0B], W=[od])
                if g == 0:
                    T.cp("dve", acc[:, :, sl], od[:], R=[od], W=[acc])
                else:
                    T.tt("dve", acc[:, :, sl], acc[:, :, sl], od[:], ALU.add, R=[od, acc], W=[acc])

            s_part(0)
            for blk in range(32):
                if blk + 1 < 32:
                    s_part(blk + 1)
                pv_part(blk)
            if g == 2:
                T.recip(acc[:, 1, :], acc[:, 1, :], R=[acc], W=[acc])
                T.tt("dve", obf[:], acc[:, 0, :], acc[:, 1, :], ALU.mult, R=[acc], W=[obf])
                T.dma("sp", oslot, k.d["oT"][hp * 128:(hp + 1) * 128, :], obf[:], R=[obf], W=[k.b_oT])
    T.barrier()


def phase_post_attn(k, layer, wo_dram, xs, xd):
    nc, T = k.nc, k.T
    x_src, x_dst, b_src, b_dst = k.d[xs], k.d[xd], k.db[xs], k.db[xd]
    with ExitStack() as es:
        wo = sb(k, es, "wo", [128, 8, D], BF16)
        T.dma("pool", T.swslot(), wo[:], wo_dram.rearrange("(kk p) n -> p kk n", p=128), W=[wo])
        gm = sb(k, es, "gm", [128, D], F32)
        gs = sb(k, es, "gsf", [128, D], F32)
        sh = sb(k, es, "shf", [128, D], F32)
        rw = sb(k, es, "rw", [128, 8, NE], F32)
        rb = sb(k, es, "rb", [128, NE], F32)
        T.dma("sp", T.slot(), gm[:], bcast_row(ada_row(k, layer, ROW_GATE_M)), R=[k.b_ada], W=[gm])
        T.dma("sp", T.slot(), gs[:], bcast_row(ada_row(k, layer, ROW_GS_F)), R=[k.b_ada], W=[gs])
        T.dma("sp", T.slot(), sh[:], bcast_row(ada_row(k, layer, ROW_SHIFT_F)), R=[k.b_ada], W=[sh])
        T.dma("sp", T.slot(), rw[:], k.d["router_w"][layer].rearrange("(kk p) n -> p kk n", p=128), W=[rw])
        T.dma("sp", T.slot(), rb[:], bcast_row(k.d["router_b"][layer:layer + 1, :]), W=[rb])
        oTt = [sb(k, es, "oTt", [128, 8, 512], BF16) for _ in range(2)]
        oq = [T.slot() for _ in range(2)]
        xt = [sb(k, es, "xt", [128, D], F32) for _ in range(2)]
        xq = [T.slot() for _ in range(2)]
        tmp = sb(k, es, "tmp", [128, D], F32)
        x1 = [sb(k, es, "x1", [128, D], F32) for _ in range(2)]
        x1q = [T.slot() for _ in range(2)]
        junk = sb(k, es, "junk", [128, D], F32)
        hf = sb(k, es, "hf", [128, D], F32)
        h2 = sb(k, es, "h2", [128, D], F32)
        ss = [sb(k, es, "ss", [128, 4], F32) for _ in range(2)]
        rstd = [sb(k, es, "rstd", [128, 1], F32) for _ in range(2)]
        h2Tf = [sb(k, es, "h2Tf", [128, 8, 128], F32) for _ in range(2)]
        h2Tb = [sb(k, es, "h2Tb", [128, 8, 512], BF16) for _ in range(2)]
        hq = [T.slot() for _ in range(2)]
        lg = sb(k, es, "lg", [128, NE], F32)
        m8 = sb(k, es, "m8", [128, 8], F32)
        sm = sb(k, es, "sm", [128, 4], F32)
        ex = sb(k, es, "ex", [128, NE], F32)
        gt = [sb(k, es, "gt", [128, NE], F32) for _ in range(2)]
        gq = [T.slot() for _ in range(2)]
        gTs = [sb(k, es, "gTs", [NE, 512], F32) for _ in range(2)]
        gTq = [T.slot() for _ in range(2)]
        pmix = [ps(k, es, "pmix", [128, D], F32) for _ in range(2)]
        ptr = ps(k, es, "ptr", [128, D], F32)
        plg = ps(k, es, "plg", [128, 128], F32)
        oview = k.d["oT"].rearrange("(kk p) t -> p kk t", p=128)
        hview = k.d["h2T"].rearrange("(kk p) t -> p kk t", p=128)
        zq = [T.slot() for _ in range(2)]
        xb_k = [Buf("xbuf_k%d" % kq) for kq in range(4)]
        carry = [sb(k, es, "pcarry", [128, NE], F32) for _ in range(2)]
        T.memset("dve", carry[0][:], 0.0, W=[carry[0]])
        pos_all = sb(k, es, "pos_all", [128, NT, NE], F32)
        g_all = sb(k, es, "g_all", [128, NT, NE], F32)
        mk = sb(k, es, "mk", [128, NE], F32)
        h2b = [sb(k, es, "h2b", [128, D], BF16) for _ in range(2)]
        h2q = [T.slot() for _ in range(2)]
        ppos = ps(k, es, "ppos", [128, 128], F32)
        def stage_a(i):
            grp, sub = divmod(i, 4)
            gp = grp % 2
            p = i % 2
            if sub == 0:
                T.dma("sp", oq[gp], oTt[gp][:], oview[:, :, grp * 512:(grp + 1) * 512], R=[k.b_oT], W=[oTt[gp]])
            T.dma("sp", xq[p], xt[p][:], x_src[i * 128:(i + 1) * 128, :], R=[b_src], W=[xt[p]])
            pm = pmix[p]
            for half in range(2):
                for kk in range(8):
                    T.mm(pm[:, half * 512:(half + 1) * 512], oTt[gp][:, kk, sub * 128:(sub + 1) * 128],
                         wo[:, kk, half * 512:(half + 1) * 512], start=(kk == 0), stop=(kk == 7),
                         R=[oTt[gp], wo], W=[pm])
            T.tt("dve", tmp[:], pm[:], gm[:], ALU.mult, R=[pm, gm], W=[tmp])
            T.tt("pool", x1[p][:], tmp[:], xt[p][:], ALU.add, R=[tmp, xt[p]], W=[x1[p]])
            T.dma("sp", x1q[p], x_dst[i * 128:(i + 1) * 128, :], x1[p][:], R=[x1[p]], W=[b_dst])
            rstd_of(k, x1[p], junk, ss[p], rstd[p])
            T.stt(hf[:], x1[p][:], rstd[p][:, 0:1], gs[:], ALU.mult, ALU.mult, R=[x1[p], rstd[p], gs], W=[hf])
            T.tt("pool", h2[:], hf[:], sh[:], ALU.add, R=[hf, sh], W=[h2])
            for j in range(8):
                T.tr(ptr[:, j * 128:(j + 1) * 128], h2[:, j * 128:(j + 1) * 128], k.ident_f[:],
                     R=[h2, k.ident_f], W=[ptr])
            T.cp("act", h2Tf[i % 2][:], ptr[:].rearrange("p (j t) -> p j t", j=8), R=[ptr], W=[h2Tf[i % 2]])
            hb_ = h2b[i % 2]
            T.cp("pool", hb_[:], h2[:], R=[h2], W=[hb_])
            T.dma("sp", h2q[i % 2], k.d["h2tm"][i * 128:(i + 1) * 128, :], hb_[:], R=[hb_], W=[k.db["h2tm"]])

        def stage_b(i):
            p = i % 2
            for kk in range(8):
                T.mm(plg[:, 0:NE], h2Tf[i % 2][:, kk, :], rw[:, kk, :], start=(kk == 0), stop=(kk == 7),
                     R=[h2Tf[i % 2], rw], W=[plg])
            T.tt("dve", lg[:], plg[:, 0:NE], rb[:], ALU.add, R=[plg, rb], W=[lg])
            T.op("dve", lambda h: h.max(m8[:], lg[:]), R=[lg], W=[m8])
            T.ts("dve", sm[:, 0:1], m8[:, 0:1], -1.0, None, ALU.mult, R=[m8], W=[sm])
            T.act(ex[:], lg[:], AF.Exp, bias=sm[:, 0:1], R=[lg, sm], W=[ex])
            g = gt[p]
            T.stt(g[:], lg[:], m8[:, 3:4], ex[:], ALU.is_ge, ALU.mult, accum=sm[:, 1:2], R=[lg, m8, ex], W=[g, sm])
            T.recip(sm[:, 2:3], sm[:, 1:2], R=[sm], W=[sm])
            T.ts("dve", g[:], g[:], sm[:, 2:3], None, ALU.mult, R=[g, sm], W=[g])
            c0, c1 = carry[i % 2], carry[(i + 1) % 2]
            T.cp("dve", g_all[:, i, :], g[:], R=[g], W=[g_all])
            T.ts("dve", mk[:], g[:], 0.0, None, ALU.is_gt, R=[g], W=[mk])
            T.mm(ppos[:, 0:NE], k.tris_f[:], mk[:], R=[k.tris_f, mk], W=[ppos])
            T.mm(ppos[:, NE:2 * NE], k.ones_f[:], mk[:], R=[k.ones_f, mk], W=[ppos])
            T.tt("dve", pos_all[:, i, :], ppos[:, 0:NE], c0[:], ALU.add, R=[ppos, c0], W=[pos_all])
            T.tt("dve", c1[:], ppos[:, NE:2 * NE], c0[:], ALU.add, R=[ppos, c0], W=[c1])

        stage_a(0)
        for i in range(NT):
            if i + 1 < NT:
                stage_a(i + 1)
            stage_b(i)
        cnt = carry[NT % 2]
        nb = sb(k, es, "nb", [128, NE], F32)
        t32 = sb(k, es, "t32", [128, NE], F32)
        z32 = sb(k, es, "z32", [128, NE], F32)
        pend = sb(k, es, "pend", [128, NE], F32)
        pst1 = sb(k, es, "pst1", [128, NE], F32)
        T.memset("dve", z32[:], 0.0, W=[z32])
        T.ts("dve", nb[:], cnt[:], 0.0, None, ALU.is_gt, R=[cnt], W=[nb])
        for m_ in range(1, S // BLK):
            T.ts("dve", t32[:], cnt[:], float(BLK * m_), None, ALU.is_gt, R=[cnt], W=[t32])
            T.tt("dve", nb[:], nb[:], t32[:], ALU.add, R=[nb, t32], W=[nb])
        T.ts("dve", nb[:], nb[:], float(BLK), None, ALU.mult, R=[nb], W=[nb])
        T.op("dve", lambda h: h.tensor_tensor_scan(pend[:], nb[:], z32[:], 0.0, ALU.add, ALU.add),
             R=[nb, z32], W=[pend])
        T.tt("dve", pst1[:], pend[:], nb[:], ALU.subtract, R=[pend, nb], W=[pst1])
        T.ts("dve", pst1[:], pst1[:], 1.0, None, ALU.add, R=[pst1], W=[pst1])
        cmp_ = sb(k, es, "cmp", [128, NBLK, NE], F32)
        ebf = sb(k, es, "ebf", [128, NBLK], F32)
        T.tt("dve", cmp_[:], pend[:].unsqueeze(1).to_broadcast([128, NBLK, NE]),
             k.bstart[:].unsqueeze(2).to_broadcast([128, NBLK, NE]), ALU.is_le, R=[pend, k.bstart], W=[cmp_])
        T.op("dve", lambda h: h.reduce_sum(ebf[:], cmp_[:], AX.X), R=[cmp_], W=[ebf])
        T.ts("dve", ebf[:], ebf[:], float(NE - 1), None, ALU.min, R=[ebf], W=[ebf])
        wif = sb(k, es, "wif", [128, NBLK, 8], F32)
        T.ts("dve", wif[:], ebf[:].unsqueeze(2).to_broadcast([128, NBLK, 8]), 1024.0, float(layer * NE * 1024),
             ALU.mult, ALU.add, R=[ebf], W=[wif])
        T.tt("dve", wif[:], wif[:], k.wbase[:].unsqueeze(1).to_broadcast([128, NBLK, 8]), ALU.add,
             R=[wif, k.wbase], W=[wif])
        T.cp("dve", k.widx[:], wif[:], R=[wif], W=[k.widx])
        T.ts("dve", wif[:, :, 0], ebf[:], 128.0, float(layer * NE * 128), ALU.mult, ALU.add, R=[ebf], W=[wif])
        T.tt("dve", wif[:, :, 0], wif[:, :, 0], k.wbase[:, 0:1].to_broadcast([128, NBLK]), ALU.add,
             R=[wif, k.wbase], W=[wif])
        T.cp("dve", k.bidx[:, :, 0], wif[:, :, 0], R=[wif], W=[k.bidx])
        T.ts("dve", wif[:, :, 1], ebf[:], float(layer * NE), None, ALU.add, R=[ebf], W=[wif])
        T.cp("dve", k.bidx[:, :, 1], wif[:, :, 1], R=[wif], W=[k.bidx])
        d1 = sb(k, es, "d1", [128, NE], F32)
        j32 = sb(k, es, "j32", [128, NE], F32)
        d8 = sb(k, es, "d8", [128, 8], F32)
        sf = sb(k, es, "sidf", [128, 8], F32)
        sidx = [sb(k, es, "sidx", [128, 4], I32) for _ in range(2)]
        hb2 = [sb(k, es, "hb2", [128, D], BF16) for _ in range(2)]
        hb2q = [T.slot() for _ in range(2)]
        scq = [T.swslot() for _ in range(4)]
        for i in range(NT):
            T.ts("dve", mk[:], g_all[:, i, :], 0.0, None, ALU.is_gt, R=[g_all], W=[mk])
            T.tt("dve", d1[:], pos_all[:, i, :], pst1[:], ALU.add, R=[pos_all, pst1], W=[d1])
            T.tt("dve", d1[:], d1[:], mk[:], ALU.mult, R=[d1, mk], W=[d1])
            T.ts("dve", d1[:], d1[:], -1.0, None, ALU.add, R=[d1], W=[d1])
            T.op("dve", lambda h: h.max(d8[:], d1[:]), R=[d1], W=[d8])
            for kq in range(4):
                T.stt(j32[:], d1[:], d8[:, kq:kq + 1], g_all[:, i, :], ALU.is_equal, ALU.mult,
                      accum=k.gsel_all[:, i, kq:kq + 1], R=[d1, d8, g_all], W=[j32, k.gsel_all])
            T.ts("dve", sf[:, 0:4], d8[:, 0:4], 0.0, 1.0e6, ALU.is_lt, ALU.mult, R=[d8], W=[sf])
            T.tt("dve", sf[:, 0:4], sf[:, 0:4], d8[:, 0:4], ALU.add, R=[sf, d8], W=[sf])
            si = sidx[i % 2]
            T.cp("dve", si[:], sf[:, 0:4], R=[sf], W=[si])
            T.ts("dve", sf[:, 4:8], d8[:, 0:4], 0.0, None, ALU.max, R=[d8], W=[sf])
            T.cp("dve", k.gidx_all[:, i, :], sf[:, 4:8], R=[sf], W=[k.gidx_all])
            hb_ = hb2[i % 2]
            T.dma("sp", hb2q[i % 2], hb_[:], k.d["h2tm"][i * 128:(i + 1) * 128, :], R=[k.db["h2tm"]], W=[hb_])
            for kq in range(4):
                T.idma(scq[kq], k.d["xbuf"], bass.IndirectOffsetOnAxis(ap=si[:, kq:kq + 1], axis=0), hb_[:], None,
                       NROWS - 1, R=[hb_, si], W=[xb_k[kq]])
    T.barrier()


MOE_TT = 512


def phase_moe(k, layer, xs, xd, final=False):
    nc, T = k.nc, k.T
    x_src, x_dst, b_src, b_dst = k.d[xs], k.d[xd], k.db[xs], k.db[xd]
    TT = MOE_TT
    NS = TT // 128
    with ExitStack() as es:
        wgu = [sb(k, es, "wgu", [128, 8, 2 * D], BF16) for _ in range(2)]
        wd = [sb(k, es, "wd", [128, 8, D], BF16) for _ in range(2)]
        wguq = [T.swslot() for _ in range(2)]
        wdq = [T.swslot() for _ in range(2)]
        bgu = sb(k, es, "bgu", [128, NE, 16], F32)
        bd = sb(k, es, "bd", [NE, D], F32)
        gf = sb(k, es, "gf", [128, D], F32)
        T.dma("sp", T.slot(), bgu[:], k.d["b_gu_col"][layer], W=[bgu])
        T.dma("sp", T.slot(), bd[:], k.d["b_down"][layer], W=[bd])
        T.dma("sp", T.slot(), gf[:], bcast_row(ada_row(k, layer, ROW_GATE_F)), R=[k.b_ada], W=[gf])
        fg = None
        if final:
            fg = sb(k, es, "fg", [128, D], F32)
            T.dma("sp", T.slot(), fg[:], bcast_row(k.d["final_norm_g"].rearrange("(o f) -> o f", o=1)), W=[fg])
        h2T = [sb(k, es, "h2Tm", [128, 8, TT], BF16) for _ in range(2)]
        h2q = [T.slot() for _ in range(2)]
        gts = [sb(k, es, "gts", [128, NS, NE], F32) for _ in range(2)]
        gtq = [T.slot() for _ in range(2)]
        gTt = [sb(k, es, "gTt", [NE, TT], F32) for _ in range(2)]
        gTq = [T.slot() for _ in range(2)]
        acc = sb(k, es, "acc", [128, NS, D], F32)
        actT = sb(k, es, "actT", [128, 8, TT], BF16)
        gc = [sb(k, es, "gc", [128, 512], F32) for _ in range(2)]
        sg = [sb(k, es, "sg", [128, 512], F32) for _ in range(2)]
        ue = [sb(k, es, "ue", [128, 512], F32) for _ in range(2)]
        wv_ = [sb(k, es, "wv", [128, 512], F32) for _ in range(2)]
        xt = [sb(k, es, "xtm", [128, D], F32) for _ in range(2)]
        xq = [T.slot() for _ in range(2)]
        xo = [sb(k, es, "xo", [128, D], F32) for _ in range(2)]
        xoq = [T.slot() for _ in range(2)]
        junk = sb(k, es, "junk", [128, D], F32) if final else None
        ss = [sb(k, es, "ss", [128, 4], F32) for _ in range(2)] if final else None
        rstd = [sb(k, es, "rstd", [128, 1], F32) for _ in range(2)] if final else None
        psg = [ps(k, es, "psg", [128, 512], F32) for _ in range(2)]
        psu = [ps(k, es, "psu", [128, 512], F32) for _ in range(2)]
        psy = [ps(k, es, "psy", [128, 512], F32) for _ in range(2)]
        hview = k.d["h2T"].rearrange("(kk p) t -> p kk t", p=128)
        gview = k.d["gates"].rearrange("(s p) e -> p s e", p=128)
        wgu_d = k.d["w_gu"][layer]
        wd_d = k.d["w_down"][layer]
        ntile = S // TT
        seq = [(t, e) for t in range(ntile) for e in range(NE)]

        def load_w(idx):
            t, e = seq[idx]
            p = idx % 2
            T.dma("pool", wguq[p], wgu[p][:], wgu_d[e].rearrange("(kk p) n -> p kk n", p=128), W=[wgu[p]])
            T.dma("pool", wdq[p], wd[p][:], wd_d[e].rearrange("(kk p) n -> p kk n", p=128), W=[wd[p]])

        def load_tile(t):
            p = t % 2
            T.dma("sp", h2q[p], h2T[p][:], hview[:, :, t * TT:(t + 1) * TT], R=[k.b_h2T], W=[h2T[p]])
            T.dma("sp", gtq[p], gts[p][:], gview[:, t * NS:(t + 1) * NS, :], R=[k.b_gates], W=[gts[p]])
            T.dma("sp", gTq[p], gTt[p][:], k.d["gatesT"][:, t * TT:(t + 1) * TT], R=[k.b_gates], W=[gTt[p]])

        load_tile(0)
        load_w(0)
        cnt = 0
        ycnt = 0
        for idx, (t, e) in enumerate(seq):
            tp = t % 2
            p = idx % 2
            if idx + 1 < len(seq):
                load_w(idx + 1)
            if e == 0:
                if t + 1 < ntile:
                    load_tile(t + 1)
                for sub in range(NS):
                    for half in range(2):
                        py = psy[ycnt % 2]
                        ycnt += 1
                        T.mm(py[:], gTt[tp][:, sub * 128:(sub + 1) * 128], bd[:, half * 512:(half + 1) * 512],
                             R=[gTt[tp], bd], W=[py])
                        T.cp("dve", acc[:, sub, half * 512:(half + 1) * 512], py[:], R=[py], W=[acc])
            W_, Wd_ = wgu[p], wd[p]
            for j in range(8):
                for tt_ in range(TT // 512):
                    c = cnt % 2
                    cnt += 1
                    tsl = slice(tt_ * 512, (tt_ + 1) * 512)
                    for kk in range(8):
                        T.mm(psg[c][:], W_[:, kk, j * 128:(j + 1) * 128], h2T[tp][:, kk, tsl],
                             start=(kk == 0), stop=(kk == 7), R=[W_, h2T[tp]], W=[psg[c]])
                    for kk in range(8):
                        T.mm(psu[c][:], W_[:, kk, D + j * 128:D + (j + 1) * 128], h2T[tp][:, kk, tsl],
                             start=(kk == 0), stop=(kk == 7), R=[W_, h2T[tp]], W=[psu[c]])
                    T.ts("dve", gc[c][:], psg[c][:], bgu[:, e, j:j + 1], 7.0, ALU.add, ALU.min,
                         R=[psg[c], bgu], W=[gc[c]])
                    T.act(sg[c][:], gc[c][:], AF.Sigmoid, scale=1.702, R=[gc[c]], W=[sg[c]])
                    T.ts("dve", ue[c][:], psu[c][:], bgu[:, e, 8 + j:9 + j], 7.0, ALU.add, ALU.min,
                         R=[psu[c], bgu], W=[ue[c]])
                    T.ts("pool", ue[c][:], ue[c][:], -7.0, 1.0, ALU.max, ALU.add, R=[ue[c]], W=[ue[c]])
                    T.tt("pool", wv_[c][:], gc[c][:], sg[c][:], ALU.mult, R=[gc[c], sg[c]], W=[wv_[c]])
                    T.tt("pool", actT[:, j, tsl], ue[c][:], wv_[c][:], ALU.mult, R=[ue[c], wv_[c]], W=[actT])
            for sub in range(NS):
                for half in range(2):
                    py = psy[ycnt % 2]
                    ycnt += 1
                    for kk in range(8):
                        T.mm(py[:], actT[:, kk, sub * 128:(sub + 1) * 128], Wd_[:, kk, half * 512:(half + 1) * 512],
                             start=(kk == 0), stop=(kk == 7), R=[actT, Wd_], W=[py])
                    a_ = acc[:, sub, half * 512:(half + 1) * 512]
                    T.stt(a_, py[:], gts[tp][:, sub, e:e + 1], a_, ALU.mult, ALU.add, R=[py, gts[tp], acc], W=[acc])
            if e == NE - 1:
                for sub in range(NS):
                    i = t * NS + sub
                    q = i % 2
                    T.dma("sp", xq[q], xt[q][:], x_src[i * 128:(i + 1) * 128, :], R=[b_src], W=[xt[q]])
                    T.tt("dve", acc[:, sub, :], acc[:, sub, :], gf[:], ALU.mult, R=[acc, gf], W=[acc])
                    T.tt("pool", xo[q][:], acc[:, sub, :], xt[q][:], ALU.add, R=[acc, xt[q]], W=[xo[q]])
                    if final:
                        rstd_of(k, xo[q], junk, ss[q], rstd[q])
                        T.stt(xo[q][:], xo[q][:], rstd[q][:, 0:1], fg[:], ALU.mult, ALU.mult,
                              R=[xo[q], rstd[q], fg], W=[xo[q]])
                    T.dma("sp", xoq[q], x_dst[i * 128:(i + 1) * 128, :], xo[q][:], R=[xo[q]], W=[b_dst])
    T.barrier()


def phase_l1_proj(k, hT, hkT, Gtm, Gref):
    nc, T = k.nc, k.T
    with ExitStack() as es:
        W1 = sb(k, es, "W1", [128, 8, D], BF16)
        wf = sb(k, es, "wf", [128, 8, 16], BF16)
        wsl = T.swslot()
        wfs = T.swslot()
        bfb = sb(k, es, "bfb", [128, 16], F32)
        T.dma("sp", T.slot(), bfb[:], bcast_row(k.d["b_f"].rearrange("(o f) -> o f", o=1)), W=[bfb])
        kvf = k.d["w_kvf"].rearrange("(kk p) n -> p kk n", p=128)
        T.dma("pool", wfs, wf[:], kvf[:, :, 2048:2064], W=[wf])
        stg = [sb(k, es, "stg", [128, S], BF16) for _ in range(2)]
        stq = [T.slot() for _ in range(2)]
        vst = [sb(k, es, "vst", [128, D], BF16) for _ in range(2)]
        vsq = [T.slot() for _ in range(2)]
        pp = [ps(k, es, "pp", [128, 512], F32) for _ in range(4)]
        pz = ps(k, es, "pz", [128, 512], F32)
        it = 0
        sidx = 0
        for name, wsrc, src in (("QT", k.d["b_w_q"].rearrange("(kk p) n -> p kk n", p=128), hT),
                                ("KT", kvf[:, :, 0:1024], hkT)):
            T.dma("pool", wsl, W1[:], wsrc, W=[W1])
            for hp in range(8):
                st = stg[sidx % 2]
                for tt_ in range(8):
                    p_ = pp[it % 4]
                    it += 1
                    for kk in range(8):
                        T.mm(p_[:], W1[:, kk, hp * 128:(hp + 1) * 128], src[:, kk, tt_ * 512:(tt_ + 1) * 512],
                             start=(kk == 0), stop=(kk == 7), R=[W1, src], W=[p_])
                    T.cp("act", st[:, tt_ * 512:(tt_ + 1) * 512], p_[:], R=[p_], W=[st])
                T.dma("sp", stq[sidx % 2], k.d[name][hp * 128:(hp + 1) * 128, :], st[:], R=[st], W=[k.db[name]])
                sidx += 1
        T.dma("pool", wsl, W1[:], kvf[:, :, 1024:2048], W=[W1])
        v2 = k.d["V2"].rearrange("h p j c -> p h j c")
        for i in range(NT):
            vs_ = vst[i % 2]
            for half in range(2):
                p_ = pp[it % 4]
                it += 1
                for kk in range(8):
                    T.mm(p_[:], hkT[:, kk, i * 128:(i + 1) * 128], W1[:, kk, half * 512:(half + 1) * 512],
                         start=(kk == 0), stop=(kk == 7), R=[hkT, W1], W=[p_])
                T.cp("act", vs_[:, half * 512:(half + 1) * 512], p_[:], R=[p_], W=[vs_])
            T.dma("sp", vsq[i % 2], v2[:, :, i, :], vs_[:].rearrange("p (h c) -> p h c", h=8), R=[vs_], W=[k.db["V2"]])
        y = sb(k, es, "y", [128, 16], F32)
        lf = sb(k, es, "lf", [128, 16], F32)
        carry = [sb(k, es, "carry", [128, 16], F32) for _ in range(2)]
        T.memset("dve", carry[0][:], 0.0, W=[carry[0]])
        for i in range(NT):
            for kk in range(8):
                T.mm(pz[:, 0:16], hkT[:, kk, i * 128:(i + 1) * 128], wf[:, kk, :], start=(kk == 0), stop=(kk == 7),
                     R=[hkT, wf], W=[pz])
            T.tt("dve", y[:], pz[:, 0:16], bfb[:], ALU.add, R=[pz, bfb], W=[y])
            T.act(y[:], y[:], AF.Exp, scale=-1.0, R=[y], W=[y])
            T.act(lf[:], y[:], AF.Ln, bias=1.0, R=[y], W=[lf])
            T.mm(pz[:, 16:32], k.tri_f[:], lf[:], R=[k.tri_f, lf], W=[pz])
            T.mm(pz[:, 32:48], k.ones_f[:], lf[:], R=[k.ones_f, lf], W=[pz])
            c0, c1 = carry[i % 2], carry[(i + 1) % 2]
            T.tt("dve", Gtm[:, i, :], pz[:, 16:32], c0[:], ALU.add, R=[pz, c0], W=[Gtm])
            T.tt("dve", c1[:], pz[:, 32:48], c0[:], ALU.add, R=[pz, c0], W=[c1])
            T.mm(pz[:, 48:64], k.sel64_f[:], Gtm[:, i, :], R=[k.sel64_f, Gtm], W=[pz])
            T.cp("dve", Gref[:, i, :], pz[:, 48:64], R=[pz], W=[Gref])
    T.barrier()


def phase_fox(k, Gtm, Gref):
    nc, T = k.nc, k.T
    with ExitStack() as es:
        QT = [sb(k, es, "fQT", [128, S], BF16) for _ in range(2)]
        KT = [sb(k, es, "fKT", [128, S], BF16) for _ in range(2)]
        Vs = [sb(k, es, "fVs", [128, 32, 128], BF16) for _ in range(2)]
        lq = [[T.slot() for _ in range(3)] for _ in range(2)]
        VA0 = sb(k, es, "fVA0", [128, 32, 128], BF16)
        V0B = sb(k, es, "fV0B", [128, 32, 128], BF16)
        T.memset("pool", VA0[:], 0.0, W=[VA0])
        T.memset("pool", V0B[:], 0.0, W=[V0B])
        Dcol = [sb(k, es, "Dcol", [128, 2, 4, 32], F32) for _ in range(2)]
        pT = [sb(k, es, "fpT", [128, 2, 512], BF16) for _ in range(3)]
        pT_b = {id(t_): [[Buf("pTr") for _ in range(4)] for _ in range(2)] for t_ in pT}
        rd = sb(k, es, "frd", [128, 512], F32)
        ob = [sb(k, es, "fob", [128, 512], BF16) for _ in range(2)]
        oq = [T.slot() for _ in range(2)]
        pS = [[ps(k, es, "fpS", [128, 512], F32) for _ in range(2)] for _ in range(2)]
        po = ps(k, es, "fpo", [128, 512], F32)
        pd = ps(k, es, "fpd", [128, 512], F32)
        pfill = ps(k, es, "fpfill", [128, 512], F32)
        NFILL = int(os.environ.get("FOX_FILL", "0"))
        v2 = k.d["V2"]

        def load(hp):
            p = hp % 2
            T.dma("sp", lq[p][0], QT[p][:], k.d["QT"][hp * 128:(hp + 1) * 128, :], R=[k.db["QT"]], W=[QT[p]])
            T.dma("sp", lq[p][1], KT[p][:], k.d["KT"][hp * 128:(hp + 1) * 128, :], R=[k.db["KT"]], W=[KT[p]])
            T.dma("sp", lq[p][2], Vs[p][:], v2[hp], R=[k.db["V2"]], W=[Vs[p]])

        load(0)
        jc = 0
        oc = 0
        for hp in range(8):
            p = hp % 2
            if hp + 1 < 8:
                load(hp + 1)
            T.cp("pool", VA0[:, :, 0:64], Vs[p][:, :, 0:64], R=[Vs[p]], W=[VA0])
            T.cp("pool", V0B[:, :, 64:128], Vs[p][:, :, 64:128], R=[Vs[p]], W=[V0B])
            for J in range(8):
                Dc = Dcol[J % 2]
                for hh in range(2):
                    h_ = hp * 2 + hh
                    for a2 in range(4):
                        a = 4 * J + a2
                        T.ts("dve", Dc[:, hh, a2, 0:a + 1], Gtm[:, 0:a + 1, h_], Gref[:, a, h_:h_ + 1], None,
                             ALU.subtract, R=[Gtm, Gref], W=[Dc])
                nj = 4 * J + 4
                st_ = {}

                def s_part(j):
                    nonlocal jc
                    a2min = max(0, j - 4 * J)
                    qs = slice(J * 512 + a2min * 128, (J + 1) * 512)
                    cs = slice(a2min * 128, 512)
                    bufs = pS[jc % 2]
                    pTb = pT[jc % 3]
                    jc += 1
                    st_[j] = (a2min, cs, pTb)
                    for hh in range(2):
                        pb = slice(hh * 64, hh * 64 + 64)
                        T.mm(bufs[hh][:, cs], KT[p][pb, j * 128:(j + 1) * 128], QT[p][pb, qs],
                             R=[KT[p], QT[p]], W=[bufs[hh]])
                    for hh in range(2):
                        for a2 in range(a2min, 4):
                            blk = slice(a2 * 128, (a2 + 1) * 128)
                            rb_ = pT_b[id(pTb)][hh][a2]
                            T.act(pTb[:, hh, blk], bufs[hh][:, blk], AF.Exp, bias=Dc[:, hh, a2, j:j + 1], scale=0.125,
                                  R=[bufs[hh], Dc], W=[rb_], same=False)
                            if j == 4 * J + a2:
                                T.tt("pool", pTb[:, hh, blk], pTb[:, hh, blk], k.mask01[:], ALU.mult,
                                     R=[rb_, k.mask01], W=[rb_])

                def pv_part(j):
                    a2min, cs, pTb = st_[j]
                    for _f in range(NFILL):
                        T.mm(pfill[:], k.onesA0[:], k.mask01w[:], R=[k.onesA0, k.mask01w], W=[pfill])
                    for hh in range(2):
                        T.mm(po[:, cs], (VA0 if hh == 0 else V0B)[:, j, :], pTb[:, hh, cs],
                             start=(j == 0 and hh == 0), stop=(j == nj - 1 and hh == 1),
                             R=[VA0, V0B] + pT_b[id(pTb)][hh][a2min:], W=[po])
                    for hh in range(2):
                        T.mm(pd[:, cs], (k.onesA0 if hh == 0 else k.ones0B)[:], pTb[:, hh, cs],
                             start=(j == 0 and hh == 0), stop=(j == nj - 1 and hh == 1),
                             R=[k.onesA0, k.ones0B] + pT_b[id(pTb)][hh][a2min:], W=[pd])

                s_part(0)
                for j in range(nj):
                    if j + 1 < nj:
                        s_part(j + 1)
                    pv_part(j)
                T.recip(rd[:], pd[:], R=[pd], W=[rd])
                o_ = ob[oc % 2]
                T.tt("dve", o_[:], po[:], rd[:], ALU.mult, R=[po, rd], W=[o_])
                T.dma("sp", oq[oc % 2], k.d["oT"][hp * 128:(hp + 1) * 128, J * 512:(J + 1) * 512], o_[:],
                      R=[o_], W=[k.b_oT])
                oc += 1
    T.barrier()


def phase_moe_sparse(k, layer, xs, xd, final=False):
    nc, T = k.nc, k.T
    x_src, x_dst, b_src, b_dst = k.d[xs], k.d[xd], k.db[xs], k.db[xd]
    NST = BLK // 128
    with ExitStack() as es:
        wgu = [sb(k, es, "wgu", [128, 8, 2 * D], BF16) for _ in range(2)]
        wd = [sb(k, es, "wd", [128, 8, D], BF16) for _ in range(2)]
        wgu_b = [[Buf("wgu%d_%d" % (p_, kk)) for kk in range(8)] for p_ in range(2)]
        wd_b = [[Buf("wd%d_%d" % (p_, kk)) for kk in range(8)] for p_ in range(2)]
        wq8 = [[T.swslot() for _ in range(8)] for _ in range(2)]
        wq8d = [[T.swslot() for _ in range(8)] for _ in range(2)]
        bgu = [sb(k, es, "bgu", [128, 16], F32) for _ in range(2)]
        bdb = [sb(k, es, "bdb", [128, D], F32) for _ in range(2)]
        bq = [T.swslot() for _ in range(2)]
        xr = [sb(k, es, "xr", [128, NST, D], BF16) for _ in range(2)]
        xrq = [T.slot() for _ in range(2)]
        xT = [sb(k, es, "xT", [128, 8, BLK], BF16) for _ in range(2)]
        actT = sb(k, es, "actT", [128, 8, BLK], BF16)
        gc = [sb(k, es, "gc", [128, BLK], F32) for _ in range(3)]
        sg = [sb(k, es, "sg", [128, BLK], F32) for _ in range(3)]
        ue = [sb(k, es, "ue", [128, BLK], F32) for _ in range(3)]
        wv_ = [sb(k, es, "wv", [128, BLK], F32) for _ in range(3)]
        yrow = [sb(k, es, "yrow", [128, D], F32) for _ in range(2)]
        yq = [T.slot() for _ in range(2)]
        psg = [ps(k, es, "psg", [128, 512], F32) for _ in range(3)]
        psu = [ps(k, es, "psu", [128, 512], F32) for _ in range(2)]
        psy = [ps(k, es, "psy", [128, 512], F32) for _ in range(2)]
        ptr = ps(k, es, "ptrm", [128, D], BF16)
        wgu_rows = k.d["w_gu"].rearrange("l e r n -> (l e r) n")
        wd_rows = k.d["w_down"].rearrange("l e r n -> (l e r) n")
        bgu_rows = k.d["b_gu_col"].rearrange("l e p j -> (l e p) j")
        bd_rows = k.d["b_down"].rearrange("l e f -> (l e) f")
        xbv = k.d["xbuf"].rearrange("(b s p) f -> b p s f", p=128, s=NST)
        ybv = k.d["ybuf"].rearrange("(b s p) f -> b s p f", p=128, s=NST)

        def off(ap_):
            return bass.IndirectOffsetOnAxis(ap=ap_, axis=0)

        def load(b):
            p = b % 2
            for kk in range(8):
                T.idma(wq8[p][kk], wgu[p][:, kk, :], None, wgu_rows, off(k.widx[:, b, kk:kk + 1]), 0,
                       R=[k.widx], W=[wgu_b[p][kk]])
            for kk in range(8):
                T.idma(wq8d[p][kk], wd[p][:, kk, :], None, wd_rows, off(k.widx[:, b, kk:kk + 1]), 0,
                       R=[k.widx], W=[wd_b[p][kk]])
            T.idma(bq[p], bgu[p][:], None, bgu_rows, off(k.bidx[:, b, 0:1]), 0, R=[k.bidx], W=[bgu[p]])
            T.idma(bq[p], bdb[p][:], None, bd_rows, off(k.bidx[:, b, 1:2]), 0, R=[k.bidx], W=[bdb[p]])
            T.dma("sp", xrq[p], xr[p][:], xbv[b], R=[k.db["xbuf"]], W=[xr[p]])

        def transposes(b):
            p = b % 2
            for st in range(NST):
                for fj in range(8):
                    T.tr(ptr[:, fj * 128:(fj + 1) * 128], xr[p][:, st, fj * 128:(fj + 1) * 128], k.ident_bf[:],
                         R=[xr[p], k.ident_bf], W=[ptr])
                T.cp("act", xT[p][:, :, st * 128:(st + 1) * 128], ptr[:].rearrange("p (j t) -> p j t", j=8),
                     R=[ptr], W=[xT[p]])

        load(0)
        transposes(0)
        cnt = 0
        ycnt = 0
        yc = 0
        for b in range(NBLK):
            p = b % 2
            if b + 1 < NBLK:
                load(b + 1)
            W_, Wd_ = wgu[p], wd[p]
            xT_ = xT[p]
            for j in range(8):
                c = cnt % 3
                c2 = cnt % 2
                cnt += 1
                for kk in range(8):
                    T.mm(psg[c][:], W_[:, kk, j * 128:(j + 1) * 128], xT_[:, kk, :],
                         start=(kk == 0), stop=(kk == 7), R=[wgu_b[p][kk], xT_], W=[psg[c]])
                for kk in range(8):
                    T.mm(psu[c2][:], W_[:, kk, D + j * 128:D + (j + 1) * 128], xT_[:, kk, :],
                         start=(kk == 0), stop=(kk == 7), R=[wgu_b[p][kk], xT_], W=[psu[c2]])
                T.ts("dve", gc[c][:], psg[c][:], bgu[p][:, j:j + 1], 7.0, ALU.add, ALU.min,
                     R=[psg[c], bgu[p]], W=[gc[c]])
                T.act(ue[c][:], psu[c2][:], AF.Identity, bias=bgu[p][:, 8 + j:9 + j], R=[psu[c2], bgu[p]], W=[ue[c]])
                T.act(sg[c][:], gc[c][:], AF.Sigmoid, scale=1.702, R=[gc[c]], W=[sg[c]])
                T.ts("dve", ue[c][:], ue[c][:], -7.0, 7.0, ALU.max, ALU.min, R=[ue[c]], W=[ue[c]])
                T.tt("dve", wv_[c][:], gc[c][:], sg[c][:], ALU.mult, R=[gc[c], sg[c]], W=[wv_[c]])
                T.stt(actT[:, j, :], ue[c][:], 1.0, wv_[c][:], ALU.add, ALU.mult, R=[ue[c], wv_[c]], W=[actT])
            if b + 1 < NBLK:
                transposes(b + 1)
            for st in range(NST):
                yr = yrow[yc % 2]
                for half in range(2):
                    py = psy[ycnt % 2]
                    ycnt += 1
                    for kk in range(8):
                        T.mm(py[:], actT[:, kk, st * 128:(st + 1) * 128], Wd_[:, kk, half * 512:(half + 1) * 512],
                             start=(kk == 0), stop=(kk == 7), R=[actT, wd_b[p][kk]], W=[py])
                    T.tt("dve", yr[:, half * 512:(half + 1) * 512], py[:], bdb[p][:, half * 512:(half + 1) * 512],
                         ALU.add, R=[py, bdb[p]], W=[yr])
                T.dma("sp", yq[yc % 2], ybv[b, st], yr[:], R=[yr], W=[k.db["ybuf"]])
                yc += 1
    T.barrier()
    with ExitStack() as es:
        gf = sb(k, es, "gf", [128, D], F32)
        T.dma("sp", T.slot(), gf[:], bcast_row(ada_row(k, layer, ROW_GATE_F)), R=[k.b_ada], W=[gf])
        fg = None
        if final:
            fg = sb(k, es, "fg", [128, D], F32)
            T.dma("sp", T.slot(), fg[:], bcast_row(k.d["final_norm_g"].rearrange("(o f) -> o f", o=1)), W=[fg])
        yg = [[sb(k, es, "yg", [128, D], F32) for _ in range(4)] for _ in range(2)]
        ygq = [[T.swslot() for _ in range(4)] for _ in range(2)]
        xt = [sb(k, es, "xtm", [128, D], F32) for _ in range(2)]
        xq = [T.slot() for _ in range(2)]
        acc = sb(k, es, "cacc", [128, D], F32)
        xo = [sb(k, es, "xo", [128, D], F32) for _ in range(2)]
        xoq = [T.slot() for _ in range(2)]
        junk = sb(k, es, "junk", [128, D], F32)
        ss = [sb(k, es, "ss", [128, 4], F32) for _ in range(2)]
        rstd = [sb(k, es, "rstd", [128, 1], F32) for _ in range(2)]

        def gather(i):
            q = i % 2
            T.dma("sp", xq[q], xt[q][:], x_src[i * 128:(i + 1) * 128, :], R=[b_src], W=[xt[q]])
            for kq in range(4):
                T.idma(ygq[q][kq], yg[q][kq][:], None, k.d["ybuf"],
                       bass.IndirectOffsetOnAxis(ap=k.gidx_all[:, i, kq:kq + 1], axis=0), NROWS - 1,
                       R=[k.gidx_all, k.db["ybuf"]], W=[yg[q][kq]])

        gather(0)
        for i in range(NT):
            q = i % 2
            if i + 1 < NT:
                gather(i + 1)
            T.ts("dve", acc[:], yg[q][0][:], k.gsel_all[:, i, 0:1], None, ALU.mult, R=[yg[q][0], k.gsel_all], W=[acc])
            for kq in range(1, 4):
                T.stt(acc[:], yg[q][kq][:], k.gsel_all[:, i, kq:kq + 1], acc[:], ALU.mult, ALU.add,
                      R=[yg[q][kq], k.gsel_all, acc], W=[acc])
            T.tt("dve", acc[:], acc[:], gf[:], ALU.mult, R=[acc, gf], W=[acc])
            T.tt("pool", xo[q][:], acc[:], xt[q][:], ALU.add, R=[acc, xt[q]], W=[xo[q]])
            if final:
                rstd_of(k, xo[q], junk, ss[q], rstd[q])
                T.stt(xo[q][:], xo[q][:], rstd[q][:, 0:1], fg[:], ALU.mult, ALU.mult, R=[xo[q], rstd[q], fg], W=[xo[q]])
            T.dma("sp", xoq[q], x_dst[i * 128:(i + 1) * 128, :], xo[q][:], R=[xo[q]], W=[b_dst])
    T.barrier()

def build(debug=False, upto=99):
    nc = bass.Bass("TRN2", target_bir_lowering=False)
    k = K()
    k.nc = nc
    k.uid = 0
    k.d = {}
    scr_kind = "ExternalOutput" if debug else "Internal"

    def din(name, shape, dt=F32):
        k.d[name] = nc.dram_tensor(name, list(shape), dt, kind="ExternalInput").ap()

    def dscr(name, shape, dt=F32):
        k.d[name] = nc.dram_tensor(name, list(shape), dt, kind=scr_kind).ap()

    din("x", [S, D])
    din("c_col", [128, 8])
    din("ada_w", [2, D, 6 * D])
    din("ada_b", [2, 6 * D])
    din("norm_mix_g", [2, D])
    din("norm_ffn_g", [2, D])
    din("a_w_qkv", [D, 9216])
    din("a_w_o", [D, D])
    din("dil_bias", [3, 8, 128, 512])
    din("dil_mask", [128, 512])
    din("kv_norm_g", [D])
    din("w_kvf", [D, 2064])
    din("b_f", [16])
    din("b_w_q", [D, D])
    din("b_w_o", [D, D])
    din("tri_f", [128, 128])
    din("ones_f", [128, 128])
    din("sel64_f", [128, 128])
    din("tris_f", [128, 128])
    din("bstart", [128, NBLK])
    din("wbase", [128, 8])
    din("mask01", [128, 128], BF16)
    din("router_w", [2, D, NE])
    din("router_b", [2, NE])
    din("w_gu", [2, NE, D, 2 * D])
    din("b_gu_col", [2, NE, 128, 16])
    din("w_down", [2, NE, D, D])
    din("b_down", [2, NE, D])
    din("final_norm_g", [D])
    din("ident_f", [128, 128])
    din("ident_bf", [128, 128], BF16)
    din("onesA0", [128, 128], BF16)
    din("ones0B", [128, 128], BF16)
    dscr("ada_scr", [12, D])
    dscr("oT", [D, S], BF16)
    dscr("h2T", [D, S], BF16)
    dscr("gates", [S, NE])
    dscr("gatesT", [NE, S])
    dscr("QT", [D, S], BF16)
    dscr("KT", [D, S], BF16)
    dscr("V2", [8, 128, 32, 128], BF16)
    dscr("xc", [S, D])
    dscr("h2tm", [S, D], BF16)
    dscr("xbuf", [NROWS, D], BF16)
    dscr("ybuf", [NROWS, D])
    dscr("xa", [S, D])
    dscr("xb", [S, D])
    k.d["out"] = nc.dram_tensor("out", [S, D], F32, kind="ExternalOutput").ap()
    k.b_x = Buf("x")
    k.b_ada = Buf("ada_scr")
    k.b_oT = Buf("oT")
    k.db = {n: Buf(n) for n in ("x", "xa", "xb", "xc", "out", "QT", "KT", "V2", "xbuf", "ybuf", "h2tm")}
    k.b_h2T = Buf("h2T")
    k.b_gates = Buf("gates")

    with ExitStack() as es:
        T = Tr(nc, es)
        k.T = T
        T.bound_reg = nc.gpsimd.to_reg(2 * NE * 1024 - 1)
        k.ident_bf = sb(k, es, "ident_bf", [128, 128], BF16)
        k.onesA0 = sb(k, es, "onesA0", [128, 128], BF16)
        k.ones0B = sb(k, es, "ones0B", [128, 128], BF16)
        k.dmask = sb(k, es, "dmask", [128, 512], F32)
        k.ident_f = sb(k, es, "ident_f", [128, 128], F32)
        q0 = T.slot()
        T.dma("sp", q0, k.ident_f[:], k.d["ident_f"], W=[k.ident_f])
        k.bstart = sb(k, es, "bstart", [128, NBLK], F32)
        T.dma("sp", q0, k.bstart[:], k.d["bstart"], W=[k.bstart])
        k.wbase = sb(k, es, "wbase", [128, 8], F32)
        T.dma("sp", q0, k.wbase[:], k.d["wbase"], W=[k.wbase])
        k.widx = sb(k, es, "widx", [128, NBLK, 8], I32)
        k.bidx = sb(k, es, "bidx", [128, NBLK, 2], I32)
        k.mask01w = sb(k, es, "mask01w", [128, 512], BF16)
        T.memset("pool", k.mask01w[:], 1.0, W=[k.mask01w])
        k.gidx_all = sb(k, es, "gidx_all", [128, NT, 4], I32)
        k.gsel_all = sb(k, es, "gsel_all", [128, NT, 4], F32)
        for nm, dt_ in (("tri_f", F32), ("ones_f", F32), ("sel64_f", F32), ("tris_f", F32), ("mask01", BF16)):
            t_ = sb(k, es, nm, [128, 128], dt_)
            setattr(k, nm, t_)
            T.dma("sp", q0, t_[:], k.d[nm], W=[t_])
        T.dma("sp", q0, k.ident_bf[:], k.d["ident_bf"], W=[k.ident_bf])
        T.dma("sp", q0, k.onesA0[:], k.d["onesA0"], W=[k.onesA0])
        T.dma("sp", q0, k.ones0B[:], k.d["ones0B"], W=[k.ones0B])
        T.dma("sp", q0, k.dmask[:], k.d["dil_mask"], W=[k.dmask])

        if upto >= 0:
            phase_ada(k)
        with ExitStack() as es1:
            if upto >= 1:
                hT, _ = phase_prenorm(k, es1, "x", 0)
            if upto >= 2:
                phase_dilated(k, hT)
        if upto >= 3:
            phase_post_attn(k, 0, k.d["a_w_o"], "x", "xa")
        if upto >= 4:
            phase_moe_sparse(k, 0, "xa", "xb")
        if upto >= 5:
            with ExitStack() as es2:
                Gtm = sb(k, es2, "Gtm", [128, NT, 16], F32)
                Gref = sb(k, es2, "Gref", [128, NT, 16], F32)
                with ExitStack() as es1:
                    hT, hkT = phase_prenorm(k, es1, "xb", 1, with_kv=True)
                    phase_l1_proj(k, hT, hkT, Gtm, Gref)
                if upto >= 6:
                    phase_fox(k, Gtm, Gref)
        if upto >= 7:
            phase_post_attn(k, 1, k.d["b_w_o"], "xb", "xc")
        if upto >= 8:
            phase_moe_sparse(k, 1, "xc", "out", final=True)
        T.barrier()
        T.barrier()
    return nc


def t5_bucket_np(n):
    max_exact = 16
    nf = np.maximum(n, 1).astype(np.float32)
    large = max_exact + (np.log(nf / np.float32(max_exact)) / np.float32(math.log(2048 / max_exact))
                         * np.float32(32 - max_exact)).astype(np.int32)
    large = np.minimum(large, 31)
    return np.where(n < max_exact, n, large)


def host_consts(rel_bias):
    kk = np.arange(128)[:, None]
    qq = np.arange(128)[None, :]
    bias = np.zeros((3, 8, 128, 2, 2, 128), np.float32)
    mask = np.zeros((128, 2, 2, 128), np.float32)
    for kc in range(2):
        delta = qq - kk + 128 * (1 - kc)
        valid = (delta >= 0) & (delta <= 128)
        mask[:, :, kc, :] = np.where(valid, 0.0, NEG)[:, None, :]
        for g, (_, d) in enumerate(DIL):
            bucket = t5_bucket_np(np.clip(delta, 0, None) * d)
            for hp in range(8):
                for hh in range(2):
                    bias[g, hp, :, hh, kc, :] = rel_bias[bucket, g * 16 + hp * 2 + hh]
    ident = np.eye(128, dtype=np.float32).astype(ml_dtypes.bfloat16)
    onesA0 = np.zeros((128, 128), np.float32)
    onesA0[:, :64] = 1
    ones0B = np.zeros((128, 128), np.float32)
    ones0B[:, 64:] = 1
    return dict(dil_bias=bias.reshape(3, 8, 128, 512), dil_mask=mask.reshape(128, 512), ident_bf=ident,
                ident_f=np.eye(128, dtype=np.float32),
                tri_f=np.triu(np.ones((128, 128), np.float32)),
                ones_f=np.ones((128, 128), np.float32),
                tris_f=np.triu(np.ones((128, 128), np.float32), 1),
                bstart=np.repeat((np.arange(NBLK, dtype=np.float32) * BLK)[None, :], 128, axis=0),
                wbase=(np.arange(8, dtype=np.float32)[None, :] * 128 + np.arange(128, dtype=np.float32)[:, None]),
                sel64_f=np.repeat((np.arange(128) == 64).astype(np.float32)[:, None], 128, axis=1),
                mask01=np.triu(np.ones((128, 128), np.float32)).astype(ml_dtypes.bfloat16),
                onesA0=onesA0.astype(ml_dtypes.bfloat16), ones0B=ones0B.astype(ml_dtypes.bfloat16))


def make_in_maps(inputs, cores):
    hc = host_consts(np.asarray(inputs["rel_bias"], np.float32))
    shared = dict(b_gu_col=np.ascontiguousarray(
        np.asarray(inputs["b_gu"]).reshape(2, NE, 16, 128).transpose(0, 1, 3, 2)))
    maps = []
    for b in cores:
        m = dict(hc)
        m["x"] = np.ascontiguousarray(inputs["x"][b])
        m["c_col"] = np.ascontiguousarray(inputs["c"][b].reshape(8, 128).T)
        m["ada_w"] = inputs["ada_w"]
        m["ada_b"] = inputs["ada_b"]
        m["norm_mix_g"] = inputs["norm_mix_g"]
        m["norm_ffn_g"] = inputs["norm_ffn_g"]
        m["a_w_qkv"] = inputs["a_w_qkv"][0]
        m["a_w_o"] = inputs["a_w_o"][0]
        m["kv_norm_g"] = inputs["kv_norm_g"]
        m["w_kvf"] = inputs["w_kvf"]
        m["b_f"] = inputs["b_f"]
        m["b_w_q"] = inputs["b_w_q"][0]
        m["b_w_o"] = inputs["b_w_o"][0]
        m["router_w"] = inputs["router_w"]
        m["router_b"] = inputs["router_b"]
        m["w_gu"] = inputs["w_gu"]
        m["w_down"] = inputs["w_down"]
        m["b_down"] = inputs["b_down"]
        m["final_norm_g"] = inputs["final_norm_g"]
        m["b_gu_col"] = shared["b_gu_col"]
        maps.append(m)
    return maps


def kernel(**inputs):
    inputs = {k_: np.asarray(v) for k_, v in inputs.items()}
    nc = build()
    maps = make_in_maps(inputs, list(range(8)))
    res = run_bass_kernel_spmd(nc, maps, core_ids=list(range(8)))
    return np.stack([r["out"] for r in res.results], axis=0)
```

```python
import math
import os
from contextlib import ExitStack

import ml_dtypes
import numpy as np

import concourse.bass as bass
import concourse.mybir as mybir
from concourse.bass_utils import run_bass_kernel_spmd

F32 = mybir.dt.float32
BF16 = mybir.dt.bfloat16
AF = mybir.ActivationFunctionType
ALU = mybir.AluOpType
AX = mybir.AxisListType

S = 4096
D = 1024
NT = S // 128
NE = 32
DIL = ((128, 1), (512, 4), (2048, 16))
NEG = -1e30
BLK = 384
NBLK = (4 * S + NE * (BLK - 1)) // BLK
NROWS = NBLK * BLK
I32 = mybir.dt.int32
DEBUG = False


class Eng:
    def __init__(self, name, sem, h):
        self.name, self.sem, self.h = name, sem, h
        self.count = 0
        self.seen = {}


class Buf:
    def __init__(self, name):
        self.name = name
        self.w = None
        self.r = {}


class Tile:
    def __init__(self, t, name):
        self.t = t
        self.b = Buf(name)

    def __getitem__(self, k):
        return self.t[k]


class Tr:
    def __init__(self, nc, es):
        self.nc, self.es = nc, es
        self.E = {}
        for n, h in (("pe", nc.tensor), ("act", nc.scalar), ("dve", nc.vector),
                     ("pool", nc.gpsimd), ("sp", nc.sync)):
            self.E[n] = Eng(n, es.enter_context(nc.semaphore("s_" + n)), h)
        self.slots = []
        self.nslot = 0
        for i in range(48):
            self.slots.append(Eng("q%d" % i, es.enter_context(nc.semaphore("q%d" % i)), None))

        self.swslots = []
        self.nsw = 0
        for i in range(40):
            self.swslots.append(Eng("w%d" % i, es.enter_context(nc.semaphore("w%d" % i)), None))

    def slot(self):
        e = self.slots[self.nslot]
        self.nslot += 1
        return e

    def swslot(self):
        e = self.swslots[self.nsw]
        self.nsw += 1
        return e

    def reset_slots(self):
        self.nslot = 0
        self.nsw = 0

    def _deps(self, e, reads, writes, same):
        deps = {}

        def add(eng, cnt):
            if deps.get(eng, 0) < cnt:
                deps[eng] = cnt

        for b in reads:
            if b.w is not None:
                add(*b.w)
        for b in writes:
            if b.w is not None:
                add(*b.w)
            for eng, cnt in b.r.items():
                add(eng, cnt)
        for eng, cnt in deps.items():
            if eng is e and not same:
                continue
            if e.seen.get(eng, 0) < cnt:
                e.h.wait_ge(eng.sem, cnt)
                e.seen[eng] = cnt

    def op(self, en, fn, R=(), W=(), same=True):
        e = self.E[en]
        R = [x.b if isinstance(x, Tile) else x for x in R]
        W = [x.b if isinstance(x, Tile) else x for x in W]
        self._deps(e, R, W, same and en != "pe")
        e.count += 1
        fn(e.h).then_inc(e.sem, 1)
        for b in R:
            b.r[e] = e.count
        for b in W:
            b.w = (e, e.count)
            b.r = {}

    def dma(self, en, slot, out, in_, R=(), W=(), **kw):
        e = self.E[en]
        R = [x.b if isinstance(x, Tile) else x for x in R]
        W = [x.b if isinstance(x, Tile) else x for x in W]
        self._deps(e, R, W, True)
        if e.seen.get(slot, 0) < slot.count:
            e.h.wait_ge(slot.sem, slot.count)
            e.seen[slot] = slot.count
        slot.count += 16
        e.h.dma_start(out=out, in_=in_, **kw).then_inc(slot.sem, 16)
        for b in R:
            b.r[slot] = slot.count
        for b in W:
            b.w = (slot, slot.count)
            b.r = {}

    def idma(self, slot, out, out_off, in_, in_off, bound, R=(), W=()):
        e = self.E["pool"]
        R = [x.b if isinstance(x, Tile) else x for x in R]
        W = [x.b if isinstance(x, Tile) else x for x in W]
        self._deps(e, R, W, True)
        if e.seen.get(slot, 0) < slot.count:
            e.h.wait_ge(slot.sem, slot.count)
            e.seen[slot] = slot.count
        slot.count += 16
        e.h.indirect_dma_start(out=out, out_offset=out_off, in_=in_, in_offset=in_off,
                               bounds_check=self.bound_reg, oob_is_err=False).then_inc(slot.sem, 16)
        for b in R:
            b.r[slot] = slot.count
        for b in W:
            b.w = (slot, slot.count)
            b.r = {}

    def wait_slots(self, en, slots):
        e = self.E[en]
        for o in slots:
            if e.seen.get(o, 0) < o.count:
                e.h.wait_ge(o.sem, o.count)
                e.seen[o] = o.count

    def barrier(self):
        allq = list(self.E.values()) + self.slots + self.swslots
        for e in self.E.values():
            for o in allq:
                if o is e or o.count == 0:
                    continue
                if e.seen.get(o, 0) < o.count:
                    e.h.wait_ge(o.sem, o.count)
                    e.seen[o] = o.count
        self.reset_slots()

    def mm(self, out, lhsT, rhs, start=True, stop=True, R=(), W=()):
        self.op("pe", lambda h: h.matmul(out, lhsT, rhs, start=start, stop=stop), R, W)

    def tr(self, out, in_, ident, R=(), W=()):
        self.op("pe", lambda h: h.transpose(out, in_, ident), R, W)

    def act(self, out, in_, func, bias=0.0, scale=1.0, accum=None, R=(), W=(), same=True):
        if accum is None:
            self.op("act", lambda h: h.activation(out, in_, func, bias=bias, scale=scale), R, W, same)
        else:
            self.op("act", lambda h: h.activation(out, in_, func, bias=bias, scale=scale, accum_out=accum), R, W)

    def tt(self, en, out, a, b, op, R=(), W=(), same=True):
        self.op(en, lambda h: h.tensor_tensor(out, a, b, op), R, W, same)

    def ts(self, en, out, a, s1, s2, op0, op1=None, R=(), W=()):
        if op1 is None:
            self.op(en, lambda h: h.tensor_scalar(out, a, s1, None, op0), R, W)
        else:
            self.op(en, lambda h: h.tensor_scalar(out, a, s1, s2, op0, op1), R, W)

    def stt(self, out, in0, scalar, in1, op0, op1, accum=None, R=(), W=()):
        if accum is None:
            self.op("dve", lambda h: h.scalar_tensor_tensor(out, in0, scalar, in1, op0, op1), R, W)
        else:
            self.op("dve", lambda h: h.scalar_tensor_tensor(out, in0, scalar, in1, op0, op1, accum_out=accum), R, W)

    def cp(self, en, out, in_, R=(), W=()):
        if en == "act":
            self.op("act", lambda h: h.copy(out, in_), R, W)
        else:
            self.op(en, lambda h: h.tensor_copy(out, in_), R, W)

    def memset(self, en, ap, val, W=()):
        self.op(en, lambda h: h.memset(ap, val), (), W)

    def recip(self, out, in_, R=(), W=()):
        self.op("dve", lambda h: h.reciprocal(out, in_), R, W)


class K:
    pass


def sb(k, es, name, shape, dt):
    k.uid += 1
    nm = "%s_%d" % (name, k.uid)
    return Tile(es.enter_context(k.nc.sbuf_tensor(nm, shape, dt)), nm)


def ps(k, es, name, shape, dt):
    k.uid += 1
    nm = "%s_%d" % (name, k.uid)
    return Tile(es.enter_context(k.nc.psum_tensor(nm, shape, dt)), nm)


def bcast_row(ap_row, n=128):
    return ap_row.partition_broadcast(n)


def phase_ada(k):
    nc, T = k.nc, k.T
    with ExitStack() as es:
        ccol = sb(k, es, "ccol", [128, 8], F32)
        w = [sb(k, es, "adaw", [128, 8, 512], F32) for _ in range(2)]
        wq = [T.slot() for _ in range(2)]
        arow = sb(k, es, "arow", [1, 12, 1024], F32)
        brow = sb(k, es, "brow", [1, 12, 1024], F32)
        grow = sb(k, es, "grow", [1, 4, 1024], F32)
        pst = [ps(k, es, "adaps", [1, 512], F32) for _ in range(2)]
        q0 = T.slot()
        T.dma("sp", q0, ccol[:], k.d["c_col"], W=[ccol])
        T.dma("sp", q0, brow[:], k.d["ada_b"].rearrange("(o l) (j f) -> o (l j) f", o=1, f=1024), W=[brow])
        T.dma("sp", q0, grow[:, 0:2, :], k.d["norm_mix_g"].rearrange("(o l) f -> o l f", o=1), W=[grow])
        T.dma("sp", q0, grow[:, 2:4, :], k.d["norm_ffn_g"].rearrange("(o l) f -> o l f", o=1), W=[grow])
        T.act(ccol[:], ccol[:], AF.Silu, R=[ccol], W=[ccol])
        it = 0
        for l in range(2):
            wv = k.d["ada_w"][l].rearrange("(kk p) n -> p kk n", p=128)
            for n in range(12):
                wb = w[it % 2]
                T.dma("sp", wq[it % 2], wb[:], wv[:, :, n * 512:(n + 1) * 512], W=[wb])
                pt = pst[it % 2]
                for kk in range(8):
                    T.mm(pt[:], ccol[:, kk:kk + 1], wb[:, kk, :], start=(kk == 0), stop=(kk == 7),
                         R=[ccol, wb], W=[pt])
                j, half = divmod(n, 2)
                r = l * 6 + j
                T.tt("dve", arow[:, r, half * 512:(half + 1) * 512], pt[:],
                     brow[:, r, half * 512:(half + 1) * 512], ALU.add, R=[pt, brow], W=[arow])
                it += 1
        for l in range(2):
            T.stt(arow[:, l * 6 + 1, :], arow[:, l * 6 + 1, :], 1.0, grow[:, l, :], ALU.add, ALU.mult,
                  R=[arow, grow], W=[arow])
            T.stt(arow[:, l * 6 + 4, :], arow[:, l * 6 + 4, :], 1.0, grow[:, 2 + l, :], ALU.add, ALU.mult,
                  R=[arow, grow], W=[arow])
        T.dma("sp", q0, k.d["ada_scr"].rearrange("(o r) f -> o r f", o=1), arow[:], R=[arow], W=[k.b_ada])
    T.barrier()


ROW_SHIFT_M, ROW_GS_M, ROW_GATE_M, ROW_SHIFT_F, ROW_GS_F, ROW_GATE_F = range(6)


def ada_row(k, l, which):
    r = l * 6 + which
    return k.d["ada_scr"][r:r + 1, :]


def rstd_a(k, xt, junk, ss):
    T = k.T
    T.stt(junk[:], xt[:], 1.0, xt[:], ALU.mult, ALU.mult, accum=ss[:, 0:1], R=[xt], W=[junk, ss])
    T.ts("dve", ss[:, 1:2], ss[:, 0:1], 1.0 / D, 1e-6, ALU.mult, ALU.add, R=[ss], W=[ss])
    T.act(ss[:, 2:3], ss[:, 1:2], AF.Sqrt, R=[ss], W=[ss])


def rstd_b(k, ss, rstd):
    k.T.recip(rstd[:, 0:1], ss[:, 2:3], R=[ss], W=[rstd])


def rstd_of(k, xt, junk, ss, rstd):
    rstd_a(k, xt, junk, ss)
    rstd_b(k, ss, rstd)


def phase_prenorm(k, es_out, xs, layer, with_kv=False):
    nc, T = k.nc, k.T
    x_src, b_src = k.d[xs], k.db[xs]
    hT = sb(k, es_out, "hT", [128, 8, S], BF16)
    hkT = sb(k, es_out, "hkT", [128, 8, S], BF16) if with_kv else None
    with ExitStack() as es:
        gs = sb(k, es, "gs", [128, D], F32)
        sh = sb(k, es, "sh", [128, D], F32)
        kg = sb(k, es, "kg", [128, D], F32) if with_kv else None
        q0 = T.slot()
        T.dma("sp", q0, gs[:], bcast_row(ada_row(k, layer, ROW_GS_M)), R=[k.b_ada], W=[gs])
        T.dma("sp", q0, sh[:], bcast_row(ada_row(k, layer, ROW_SHIFT_M)), R=[k.b_ada], W=[sh])
        if with_kv:
            T.dma("sp", q0, kg[:], bcast_row(k.d["kv_norm_g"].rearrange("(o f) -> o f", o=1)), W=[kg])
        xt = [sb(k, es, "xt", [128, D], F32) for _ in range(2)]
        xq = [T.slot() for _ in range(2)]
        junk = sb(k, es, "junk", [128, D], F32)
        hf = sb(k, es, "hf", [128, D], F32)
        hb = [sb(k, es, "hb", [128, D], BF16) for _ in range(2)]
        hkb = [sb(k, es, "hkb", [128, D], BF16) for _ in range(2)] if with_kv else None
        ss = [sb(k, es, "ss", [128, 4], F32) for _ in range(2)]
        rstd = [sb(k, es, "rstd", [128, 1], F32) for _ in range(2)]
        pst = [ps(k, es, "pst", [128, D], BF16) for _ in range(2)]
        pst2 = [ps(k, es, "pst2", [128, D], BF16) for _ in range(2)] if with_kv else None
        ident = k.ident_bf
        def s1(i):
            p = i % 2
            T.dma("sp", xq[p], xt[p][:], x_src[i * 128:(i + 1) * 128, :], R=[b_src], W=[xt[p]])
            rstd_a(k, xt[p], junk, ss[p])

        def s2(i):
            p = i % 2
            rstd_b(k, ss[p], rstd[p])
            T.stt(hf[:], xt[p][:], rstd[p][:, 0:1], gs[:], ALU.mult, ALU.mult, R=[xt[p], rstd[p], gs], W=[hf])
            T.tt("pool", hb[p][:], hf[:], sh[:], ALU.add, R=[hf, sh], W=[hb[p]])
            for j in range(8):
                T.tr(pst[p][:, j * 128:(j + 1) * 128], hb[p][:, j * 128:(j + 1) * 128], ident[:],
                     R=[hb[p], ident], W=[pst[p]])
            T.cp("act", hT[:, :, i * 128:(i + 1) * 128], pst[p][:].rearrange("p (j t) -> p j t", j=8),
                 R=[pst[p]], W=[hT])
            if with_kv:
                T.stt(hkb[p][:], xt[p][:], rstd[p][:, 0:1], kg[:], ALU.mult, ALU.mult,
                      R=[xt[p], rstd[p], kg], W=[hkb[p]])
                for j in range(8):
                    T.tr(pst2[p][:, j * 128:(j + 1) * 128], hkb[p][:, j * 128:(j + 1) * 128], ident[:],
                         R=[hkb[p], ident], W=[pst2[p]])
                T.cp("act", hkT[:, :, i * 128:(i + 1) * 128], pst2[p][:].rearrange("p (j t) -> p j t", j=8),
                     R=[pst2[p]], W=[hkT])

        s1(0)
        for i in range(NT):
            if i + 1 < NT:
                s1(i + 1)
            s2(i)
    T.barrier()
    return hT, hkT


def tok_slice(d, blk):
    nb = 32 // d
    r, b = divmod(blk, nb)
    start = d * 128 * b + r
    return slice(start, start + d * 127 + 1, d), b


def phase_dilated(k, hT):
    nc, T = k.nc, k.T
    with ExitStack() as es:
        wqkv = [[sb(k, es, "wqkv", [128, 8, 128], BF16) for _ in range(3)] for _ in range(2)]
        wslot = [T.swslot() for _ in range(2)]
        QT = sb(k, es, "QT", [128, S], BF16)
        KT = sb(k, es, "KT", [128, S], BF16)
        VA0 = sb(k, es, "VA0", [128, 32, 128], BF16)
        V0B = sb(k, es, "V0B", [128, 32, 128], BF16)
        bm = [sb(k, es, "bm", [128, 512], F32) for _ in range(2)]
        bslot = [T.slot() for _ in range(2)]
        acc = sb(k, es, "acc", [128, 2, S], F32)
        obf = sb(k, es, "obf", [128, S], BF16)
        oslot = T.slot()
        sf = [sb(k, es, "sf", [128, 512], F32) for _ in range(2)]
        pT = [sb(k, es, "pT", [128, 512], BF16) for _ in range(2)]
        pqk = [ps(k, es, "pqk", [128, 512], F32) for _ in range(2)]
        pvt = pqk[1]
        pss2 = [ps(k, es, "pss", [128, 2, 512], F32) for _ in range(2)]
        pod = [ps(k, es, "pod", [128, 2, 128], F32) for _ in range(2)]
        T.memset("pool", VA0[:], 0.0, W=[VA0])
        T.memset("pool", V0B[:], 0.0, W=[V0B])
        wview = k.d["a_w_qkv"].rearrange("(kk p) n -> p kk n", p=128)
        order = [(hp, g) for hp in range(8) for g in range(3)]
        import os
        order = order[:int(os.environ.get("DIL_N", "24"))]
        dstop = int(os.environ.get("DIL_STOP", "9"))

        def load_w(idx):
            hp, g = order[idx]
            p = idx % 2
            for j in range(3):
                c0 = g * 3072 + j * 1024 + hp * 128
                T.dma("pool", wslot[p], wqkv[p][j][:], wview[:, :, c0:c0 + 128], W=[wqkv[p][j]])
            T.dma("sp", bslot[p], bm[p][:], k.d["dil_bias"][g, hp], W=[bm[p]])
            T.tt("pool", bm[p][:], bm[p][:], k.dmask[:], ALU.add, R=[bm[p], k.dmask], W=[bm[p]])

        load_w(0)
        it = 0
        for idx, (hp, g) in enumerate(order):
            p = idx % 2
            if idx + 1 < len(order):
                load_w(idx + 1)
            wq_, wk_, wv_ = wqkv[p]
            d = DIL[g][1]
            if dstop < 1:
                continue
            for dst, wt in (() if os.environ.get('DIL_SKIPQK') else ((QT, wq_), (KT, wk_))):
                for tt_ in range(8):
                    pp = pqk[it % 2]
                    it += 1
                    for kk in range(8):
                        T.mm(pp[:], wt[:, kk, :], hT[:, kk, tt_ * 512:(tt_ + 1) * 512],
                             start=(kk == 0), stop=(kk == 7), R=[wt, hT], W=[pp])
                    T.cp("act", dst[:, tt_ * 512:(tt_ + 1) * 512], pp[:], R=[pp], W=[dst])
            if dstop < 2:
                continue
            for b4 in range(8):
                for bi in range(4):
                    blk = b4 * 4 + bi
                    sl, _ = tok_slice(d, blk)
                    for kk in range(8):
                        T.mm(pvt[:, bi * 128:(bi + 1) * 128], hT[:, kk, sl], wv_[:, kk, :], start=(kk == 0), stop=(kk == 7),
                             R=[hT, wv_], W=[pvt])
                pv3 = pvt[:].rearrange("p (a c) -> p a c", a=4)
                T.cp("act", VA0[:, b4 * 4:(b4 + 1) * 4, 0:64], pv3[:, :, 0:64], R=[pvt], W=[VA0])
                T.cp("dve", V0B[:, b4 * 4:(b4 + 1) * 4, 64:128], pv3[:, :, 64:128], R=[pvt, VA0], W=[V0B])
            if dstop < 3:
                continue
            def s_part(blk):
                sl, b = tok_slice(d, blk)
                slp, _ = tok_slice(d, blk - 1) if b > 0 else (None, None)
                c0 = 0
                pss = pss2[blk % 2]
                s_b = pss
                for hh in range(2):
                    pb = slice(hh * 64, hh * 64 + 64)
                    if b > 0:
                        T.mm(pss[:, hh, c0:c0 + 128], KT[pb, slp], QT[pb, sl], R=[KT, QT], W=[s_b])
                    T.mm(pss[:, hh, c0 + 128:c0 + 256], KT[pb, sl], QT[pb, sl], R=[KT, QT], W=[s_b])
                sfb, pTb = sf[blk % 2], pT[blk % 2]
                v3 = lambda t: t[:].rearrange("p (h cq) -> p h cq", h=2)
                if b > 0:
                    T.stt(v3(sfb), pss[:, :, c0:c0 + 256], 0.125, v3(bm[p]), ALU.mult, ALU.add,
                          R=[s_b, bm[p]], W=[sfb])
                    T.act(pTb[:], sfb[:], AF.Exp, R=[sfb], W=[pTb])
                else:
                    T.stt(v3(sfb)[:, :, 128:256], pss[:, :, c0 + 128:c0 + 256], 0.125, v3(bm[p])[:, :, 128:256],
                          ALU.mult, ALU.add, R=[s_b, bm[p]], W=[sfb])
                    T.act(v3(pTb)[:, :, 128:256], v3(sfb)[:, :, 128:256], AF.Exp, R=[sfb], W=[pTb])

            def pv_part(blk):
                sl, b = tok_slice(d, blk)
                pTb = pT[blk % 2]
                od = pod[blk % 2]
                for which in range(2):
                    mms = []
                    for hh in range(2):
                        for kc in range(2):
                            if kc == 0 and b == 0:
                                continue
                            vb = blk - 1 if kc == 0 else blk
                            if which == 0:
                                lhs = (VA0 if hh == 0 else V0B)[:, vb, :]
                            else:
                                lhs = (k.onesA0 if hh == 0 else k.ones0B)[:]
                            mms.append((lhs, pTb[:, (hh * 2 + kc) * 128:(hh * 2 + kc + 1) * 128]))
                    for mi, (lhs, rhs) in enumerate(mms):
                        T.mm(od[:, which, :], lhs, rhs, start=(mi == 0), stop=(mi == len(mms) - 1),
                             R=[VA0, V0B, pTb, k.onesA0, k.ones0B], W=[od])
                if g == 0:
                    T.cp("dve", acc[:, :, sl], od[:], R=[od], W=[acc])
                else:
                    T.tt("dve", acc[:, :, sl], acc[:, :, sl], od[:], ALU.add, R=[od, acc], W=[acc])

            s_part(0)
            for blk in range(32):
                if blk + 1 < 32:
                    s_part(blk + 1)
                pv_part(blk)
            if g == 2:
                T.recip(acc[:, 1, :], acc[:, 1, :], R=[acc], W=[acc])
                T.tt("dve", obf[:], acc[:, 0, :], acc[:, 1, :], ALU.mult, R=[acc], W=[obf])
                T.dma("sp", oslot, k.d["oT"][hp * 128:(hp + 1) * 128, :], obf[:], R=[obf], W=[k.b_oT])
    T.barrier()


def phase_post_attn(k, layer, wo_dram, xs, xd):
    nc, T = k.nc, k.T
    x_src, x_dst, b_src, b_dst = k.d[xs], k.d[xd], k.db[xs], k.db[xd]
    with ExitStack() as es:
        wo = sb(k, es, "wo", [128, 8, D], BF16)
        T.dma("pool", T.swslot(), wo[:], wo_dram.rearrange("(kk p) n -> p kk n", p=128), W=[wo])
        gm = sb(k, es, "gm", [128, D], F32)
        gs = sb(k, es, "gsf", [128, D], F32)
        sh = sb(k, es, "shf", [128, D], F32)
        rw = sb(k, es, "rw", [128, 8, NE], F32)
        rb = sb(k, es, "rb", [128, NE], F32)
        T.dma("sp", T.slot(), gm[:], bcast_row(ada_row(k, layer, ROW_GATE_M)), R=[k.b_ada], W=[gm])
        T.dma("sp", T.slot(), gs[:], bcast_row(ada_row(k, layer, ROW_GS_F)), R=[k.b_ada], W=[gs])
        T.dma("sp", T.slot(), sh[:], bcast_row(ada_row(k, layer, ROW_SHIFT_F)), R=[k.b_ada], W=[sh])
        T.dma("sp", T.slot(), rw[:], k.d["router_w"][layer].rearrange("(kk p) n -> p kk n", p=128), W=[rw])
        T.dma("sp", T.slot(), rb[:], bcast_row(k.d["router_b"][layer:layer + 1, :]), W=[rb])
        oTt = [sb(k, es, "oTt", [128, 8, 512], BF16) for _ in range(2)]
        oq = [T.slot() for _ in range(2)]
        xt = [sb(k, es, "xt", [128, D], F32) for _ in range(2)]
        xq = [T.slot() for _ in range(2)]
        tmp = sb(k, es, "tmp", [128, D], F32)
        x1 = [sb(k, es, "x1", [128, D], F32) for _ in range(2)]
        x1q = [T.slot() for _ in range(2)]
        junk = sb(k, es, "junk", [128, D], F32)
        hf = sb(k, es, "hf", [128, D], F32)
        h2 = sb(k, es, "h2", [128, D], F32)
        ss = [sb(k, es, "ss", [128, 4], F32) for _ in range(2)]
        rstd = [sb(k, es, "rstd", [128, 1], F32) for _ in range(2)]
        h2Tf = [sb(k, es, "h2Tf", [128, 8, 128], F32) for _ in range(2)]
        h2Tb = [sb(k, es, "h2Tb", [128, 8, 512], BF16) for _ in range(2)]
        hq = [T.slot() for _ in range(2)]
        lg = sb(k, es, "lg", [128, NE], F32)
        m8 = sb(k, es, "m8", [128, 8], F32)
        sm = sb(k, es, "sm", [128, 4], F32)
        ex = sb(k, es, "ex", [128, NE], F32)
        gt = [sb(k, es, "gt", [128, NE], F32) for _ in range(2)]
        gq = [T.slot() for _ in range(2)]
        gTs = [sb(k, es, "gTs", [NE, 512], F32) for _ in range(2)]
        gTq = [T.slot() for _ in range(2)]
        pmix = [ps(k, es, "pmix", [128, D], F32) for _ in range(2)]
        ptr = ps(k, es, "ptr", [128, D], F32)
        plg = ps(k, es, "plg", [128, 128], F32)
        oview = k.d["oT"].rearrange("(kk p) t -> p kk t", p=128)
        hview = k.d["h2T"].rearrange("(kk p) t -> p kk t", p=128)
        zq = [T.slot() for _ in range(2)]
        xb_k = [Buf("xbuf_k%d" % kq) for kq in range(4)]
        carry = [sb(k, es, "pcarry", [128, NE], F32) for _ in range(2)]
        T.memset("dve", carry[0][:], 0.0, W=[carry[0]])
        pos_all = sb(k, es, "pos_all", [128, NT, NE], F32)
        g_all = sb(k, es, "g_all", [128, NT, NE], F32)
        mk = sb(k, es, "mk", [128, NE], F32)
        h2b = [sb(k, es, "h2b", [128, D], BF16) for _ in range(2)]
        h2q = [T.slot() for _ in range(2)]
        ppos = ps(k, es, "ppos", [128, 128], F32)
        def stage_a(i):
            grp, sub = divmod(i, 4)
            gp = grp % 2
            p = i % 2
            if sub == 0:
                T.dma("sp", oq[gp], oTt[gp][:], oview[:, :, grp * 512:(grp + 1) * 512], R=[k.b_oT], W=[oTt[gp]])
            T.dma("sp", xq[p], xt[p][:], x_src[i * 128:(i + 1) * 128, :], R=[b_src], W=[xt[p]])
            pm = pmix[p]
            for half in range(2):
                for kk in range(8):
                    T.mm(pm[:, half * 512:(half + 1) * 512], oTt[gp][:, kk, sub * 128:(sub + 1) * 128],
                         wo[:, kk, half * 512:(half + 1) * 512], start=(kk == 0), stop=(kk == 7),
                         R=[oTt[gp], wo], W=[pm])
            T.tt("dve", tmp[:], pm[:], gm[:], ALU.mult, R=[pm, gm], W=[tmp])
            T.tt("pool", x1[p][:], tmp[:], xt[p][:], ALU.add, R=[tmp, xt[p]], W=[x1[p]])
            T.dma("sp", x1q[p], x_dst[i * 128:(i + 1) * 128, :], x1[p][:], R=[x1[p]], W=[b_dst])
            rstd_of(k, x1[p], junk, ss[p], rstd[p])
            T.stt(hf[:], x1[p][:], rstd[p][:, 0:1], gs[:], ALU.mult, ALU.mult, R=[x1[p], rstd[p], gs], W=[hf])
            T.tt("pool", h2[:], hf[:], sh[:], ALU.add, R=[hf, sh], W=[h2])
            for j in range(8):
                T.tr(ptr[:, j * 128:(j + 1) * 128], h2[:, j * 128:(j + 1) * 128], k.ident_f[:],
                     R=[h2, k.ident_f], W=[ptr])
            T.cp("act", h2Tf[i % 2][:], ptr[:].rearrange("p (j t) -> p j t", j=8), R=[ptr], W=[h2Tf[i % 2]])
            hb_ = h2b[i % 2]
            T.cp("pool", hb_[:], h2[:], R=[h2], W=[hb_])
            T.dma("sp", h2q[i % 2], k.d["h2tm"][i * 128:(i + 1) * 128, :], hb_[:], R=[hb_], W=[k.db["h2tm"]])

        def stage_b(i):
            p = i % 2
            for kk in range(8):
                T.mm(plg[:, 0:NE], h2Tf[i % 2][:, kk, :], rw[:, kk, :], start=(kk == 0), stop=(kk == 7),
                     R=[h2Tf[i % 2], rw], W=[plg])
            T.tt("dve", lg[:], plg[:, 0:NE], rb[:], ALU.add, R=[plg, rb], W=[lg])
            T.op("dve", lambda h: h.max(m8[:], lg[:]), R=[lg], W=[m8])
            T.ts("dve", sm[:, 0:1], m8[:, 0:1], -1.0, None, ALU.mult, R=[m8], W=[sm])
            T.act(ex[:], lg[:], AF.Exp, bias=sm[:, 0:1], R=[lg, sm], W=[ex])
            g = gt[p]
            T.stt(g[:], lg[:], m8[:, 3:4], ex[:], ALU.is_ge, ALU.mult, accum=sm[:, 1:2], R=[lg, m8, ex], W=[g, sm])
            T.recip(sm[:, 2:3], sm[:, 1:2], R=[sm], W=[sm])
            T.ts("dve", g[:], g[:], sm[:, 2:3], None, ALU.mult, R=[g, sm], W=[g])
            c0, c1 = carry[i % 2], carry[(i + 1) % 2]
            T.cp("dve", g_all[:, i, :], g[:], R=[g], W=[g_all])
            T.ts("dve", mk[:], g[:], 0.0, None, ALU.is_gt, R=[g], W=[mk])
            T.mm(ppos[:, 0:NE], k.tris_f[:], mk[:], R=[k.tris_f, mk], W=[ppos])
            T.mm(ppos[:, NE:2 * NE], k.ones_f[:], mk[:], R=[k.ones_f, mk], W=[ppos])
            T.tt("dve", pos_all[:, i, :], ppos[:, 0:NE], c0[:], ALU.add, R=[ppos, c0], W=[pos_all])
            T.tt("dve", c1[:], ppos[:, NE:2 * NE], c0[:], ALU.add, R=[ppos, c0], W=[c1])

        stage_a(0)
        for i in range(NT):
            if i + 1 < NT:
                stage_a(i + 1)
            stage_b(i)
        cnt = carry[NT % 2]
        nb = sb(k, es, "nb", [128, NE], F32)
        t32 = sb(k, es, "t32", [128, NE], F32)
        z32 = sb(k, es, "z32", [128, NE], F32)
        pend = sb(k, es, "pend", [128, NE], F32)
        pst1 = sb(k, es, "pst1", [128, NE], F32)
        T.memset("dve", z32[:], 0.0, W=[z32])
        T.ts("dve", nb[:], cnt[:], 0.0, None, ALU.is_gt, R=[cnt], W=[nb])
        for m_ in range(1, -(-S // BLK)):
            T.ts("dve", t32[:], cnt[:], float(BLK * m_), None, ALU.is_gt, R=[cnt], W=[t32])
            T.tt("dve", nb[:], nb[:], t32[:], ALU.add, R=[nb, t32], W=[nb])
        T.ts("dve", nb[:], nb[:], float(BLK), None, ALU.mult, R=[nb], W=[nb])
        T.op("dve", lambda h: h.tensor_tensor_scan(pend[:], nb[:], z32[:], 0.0, ALU.add, ALU.add),
             R=[nb, z32], W=[pend])
        T.tt("dve", pst1[:], pend[:], nb[:], ALU.subtract, R=[pend, nb], W=[pst1])
        T.ts("dve", pst1[:], pst1[:], 1.0, None, ALU.add, R=[pst1], W=[pst1])
        cmp_ = sb(k, es, "cmp", [128, NBLK, NE], F32)
        ebf = sb(k, es, "ebf", [128, NBLK], F32)
        T.tt("dve", cmp_[:], pend[:].unsqueeze(1).to_broadcast([128, NBLK, NE]),
             k.bstart[:].unsqueeze(2).to_broadcast([128, NBLK, NE]), ALU.is_le, R=[pend, k.bstart], W=[cmp_])
        T.op("dve", lambda h: h.reduce_sum(ebf[:], cmp_[:], AX.X), R=[cmp_], W=[ebf])
        T.ts("dve", ebf[:], ebf[:], float(NE - 1), None, ALU.min, R=[ebf], W=[ebf])
        wif = sb(k, es, "wif", [128, NBLK, 8], F32)
        T.ts("dve", wif[:], ebf[:].unsqueeze(2).to_broadcast([128, NBLK, 8]), 1024.0, float(layer * NE * 1024),
             ALU.mult, ALU.add, R=[ebf], W=[wif])
        T.tt("dve", wif[:], wif[:], k.wbase[:].unsqueeze(1).to_broadcast([128, NBLK, 8]), ALU.add,
             R=[wif, k.wbase], W=[wif])
        T.cp("dve", k.widx[:], wif[:], R=[wif], W=[k.widx])
        T.ts("dve", wif[:, :, 0], ebf[:], 128.0, float(layer * NE * 128), ALU.mult, ALU.add, R=[ebf], W=[wif])
        T.tt("dve", wif[:, :, 0], wif[:, :, 0], k.wbase[:, 0:1].to_broadcast([128, NBLK]), ALU.add,
             R=[wif, k.wbase], W=[wif])
        T.cp("dve", k.bidx[:, :, 0], wif[:, :, 0], R=[wif], W=[k.bidx])
        T.ts("dve", wif[:, :, 1], ebf[:], float(layer * NE), None, ALU.add, R=[ebf], W=[wif])
        T.cp("dve", k.bidx[:, :, 1], wif[:, :, 1], R=[wif], W=[k.bidx])
        d1 = sb(k, es, "d1", [128, NE], F32)
        j32 = sb(k, es, "j32", [128, NE], F32)
        d8 = sb(k, es, "d8", [128, 8], F32)
        sf = sb(k, es, "sidf", [128, 8], F32)
        sidx = [sb(k, es, "sidx", [128, 4], I32) for _ in range(2)]
        hb2 = [sb(k, es, "hb2", [128, D], BF16) for _ in range(2)]
        hb2q = [T.slot() for _ in range(2)]
        scq = [T.swslot() for _ in range(4)]
        for i in range(NT):
            T.ts("dve", mk[:], g_all[:, i, :], 0.0, None, ALU.is_gt, R=[g_all], W=[mk])
            T.tt("dve", d1[:], pos_all[:, i, :], pst1[:], ALU.add, R=[pos_all, pst1], W=[d1])
            T.tt("dve", d1[:], d1[:], mk[:], ALU.mult, R=[d1, mk], W=[d1])
            T.ts("dve", d1[:], d1[:], -1.0, None, ALU.add, R=[d1], W=[d1])
            T.op("dve", lambda h: h.max(d8[:], d1[:]), R=[d1], W=[d8])
            for kq in range(4):
                T.stt(j32[:], d1[:], d8[:, kq:kq + 1], g_all[:, i, :], ALU.is_equal, ALU.mult,
                      accum=k.gsel_all[:, i, kq:kq + 1], R=[d1, d8, g_all], W=[j32, k.gsel_all])
            T.ts("dve", sf[:, 0:4], d8[:, 0:4], 0.0, 1.0e6, ALU.is_lt, ALU.mult, R=[d8], W=[sf])
            T.tt("dve", sf[:, 0:4], sf[:, 0:4], d8[:, 0:4], ALU.add, R=[sf, d8], W=[sf])
            si = sidx[i % 2]
            T.cp("dve", si[:], sf[:, 0:4], R=[sf], W=[si])
            T.ts("dve", sf[:, 4:8], d8[:, 0:4], 0.0, None, ALU.max, R=[d8], W=[sf])
            T.cp("dve", k.gidx_all[:, i, :], sf[:, 4:8], R=[sf], W=[k.gidx_all])
            hb_ = hb2[i % 2]
            T.dma("sp", hb2q[i % 2], hb_[:], k.d["h2tm"][i * 128:(i + 1) * 128, :], R=[k.db["h2tm"]], W=[hb_])
            for kq in range(4):
                T.idma(scq[kq], k.d["xbuf"], bass.IndirectOffsetOnAxis(ap=si[:, kq:kq + 1], axis=0), hb_[:], None,
                       NROWS - 1, R=[hb_, si], W=[xb_k[kq]])
    T.barrier()


MOE_TT = 512


def phase_moe(k, layer, xs, xd, final=False):
    nc, T = k.nc, k.T
    x_src, x_dst, b_src, b_dst = k.d[xs], k.d[xd], k.db[xs], k.db[xd]
    TT = MOE_TT
    NS = TT // 128
    with ExitStack() as es:
        wgu = [sb(k, es, "wgu", [128, 8, 2 * D], BF16) for _ in range(2)]
        wd = [sb(k, es, "wd", [128, 8, D], BF16) for _ in range(2)]
        wguq = [T.swslot() for _ in range(2)]
        wdq = [T.swslot() for _ in range(2)]
        bgu = sb(k, es, "bgu", [128, NE, 16], F32)
        bd = sb(k, es, "bd", [NE, D], F32)
        gf = sb(k, es, "gf", [128, D], F32)
        T.dma("sp", T.slot(), bgu[:], k.d["b_gu_col"][layer], W=[bgu])
        T.dma("sp", T.slot(), bd[:], k.d["b_down"][layer], W=[bd])
        T.dma("sp", T.slot(), gf[:], bcast_row(ada_row(k, layer, ROW_GATE_F)), R=[k.b_ada], W=[gf])
        fg = None
        if final:
            fg = sb(k, es, "fg", [128, D], F32)
            T.dma("sp", T.slot(), fg[:], bcast_row(k.d["final_norm_g"].rearrange("(o f) -> o f", o=1)), W=[fg])
        h2T = [sb(k, es, "h2Tm", [128, 8, TT], BF16) for _ in range(2)]
        h2q = [T.slot() for _ in range(2)]
        gts = [sb(k, es, "gts", [128, NS, NE], F32) for _ in range(2)]
        gtq = [T.slot() for _ in range(2)]
        gTt = [sb(k, es, "gTt", [NE, TT], F32) for _ in range(2)]
        gTq = [T.slot() for _ in range(2)]
        acc = sb(k, es, "acc", [128, NS, D], F32)
        actT = sb(k, es, "actT", [128, 8, TT], BF16)
        gc = [sb(k, es, "gc", [128, 512], F32) for _ in range(2)]
        sg = [sb(k, es, "sg", [128, 512], F32) for _ in range(2)]
        ue = [sb(k, es, "ue", [128, 512], F32) for _ in range(2)]
        wv_ = [sb(k, es, "wv", [128, 512], F32) for _ in range(2)]
        xt = [sb(k, es, "xtm", [128, D], F32) for _ in range(2)]
        xq = [T.slot() for _ in range(2)]
        xo = [sb(k, es, "xo", [128, D], F32) for _ in range(2)]
        xoq = [T.slot() for _ in range(2)]
        junk = sb(k, es, "junk", [128, D], F32) if final else None
        ss = [sb(k, es, "ss", [128, 4], F32) for _ in range(2)] if final else None
        rstd = [sb(k, es, "rstd", [128, 1], F32) for _ in range(2)] if final else None
        psg = [ps(k, es, "psg", [128, 512], F32) for _ in range(2)]
        psu = [ps(k, es, "psu", [128, 512], F32) for _ in range(2)]
        psy = [ps(k, es, "psy", [128, 512], F32) for _ in range(2)]
        hview = k.d["h2T"].rearrange("(kk p) t -> p kk t", p=128)
        gview = k.d["gates"].rearrange("(s p) e -> p s e", p=128)
        wgu_d = k.d["w_gu"][layer]
        wd_d = k.d["w_down"][layer]
        ntile = S // TT
        seq = [(t, e) for t in range(ntile) for e in range(NE)]

        def load_w(idx):
            t, e = seq[idx]
            p = idx % 2
            T.dma("pool", wguq[p], wgu[p][:], wgu_d[e].rearrange("(kk p) n -> p kk n", p=128), W=[wgu[p]])
            T.dma("pool", wdq[p], wd[p][:], wd_d[e].rearrange("(kk p) n -> p kk n", p=128), W=[wd[p]])

        def load_tile(t):
            p = t % 2
            T.dma("sp", h2q[p], h2T[p][:], hview[:, :, t * TT:(t + 1) * TT], R=[k.b_h2T], W=[h2T[p]])
            T.dma("sp", gtq[p], gts[p][:], gview[:, t * NS:(t + 1) * NS, :], R=[k.b_gates], W=[gts[p]])
            T.dma("sp", gTq[p], gTt[p][:], k.d["gatesT"][:, t * TT:(t + 1) * TT], R=[k.b_gates], W=[gTt[p]])

        load_tile(0)
        load_w(0)
        cnt = 0
        ycnt = 0
        for idx, (t, e) in enumerate(seq):
            tp = t % 2
            p = idx % 2
            if idx + 1 < len(seq):
                load_w(idx + 1)
            if e == 0:
                if t + 1 < ntile:
                    load_tile(t + 1)
                for sub in range(NS):
                    for half in range(2):
                        py = psy[ycnt % 2]
                        ycnt += 1
                        T.mm(py[:], gTt[tp][:, sub * 128:(sub + 1) * 128], bd[:, half * 512:(half + 1) * 512],
                             R=[gTt[tp], bd], W=[py])
                        T.cp("dve", acc[:, sub, half * 512:(half + 1) * 512], py[:], R=[py], W=[acc])
            W_, Wd_ = wgu[p], wd[p]
            for j in range(8):
                for tt_ in range(TT // 512):
                    c = cnt % 2
                    cnt += 1
                    tsl = slice(tt_ * 512, (tt_ + 1) * 512)
                    for kk in range(8):
                        T.mm(psg[c][:], W_[:, kk, j * 128:(j + 1) * 128], h2T[tp][:, kk, tsl],
                             start=(kk == 0), stop=(kk == 7), R=[W_, h2T[tp]], W=[psg[c]])
                    for kk in range(8):
                        T.mm(psu[c][:], W_[:, kk, D + j * 128:D + (j + 1) * 128], h2T[tp][:, kk, tsl],
                             start=(kk == 0), stop=(kk == 7), R=[W_, h2T[tp]], W=[psu[c]])
                    T.ts("dve", gc[c][:], psg[c][:], bgu[:, e, j:j + 1], 7.0, ALU.add, ALU.min,
                         R=[psg[c], bgu], W=[gc[c]])
                    T.act(sg[c][:], gc[c][:], AF.Sigmoid, scale=1.702, R=[gc[c]], W=[sg[c]])
                    T.ts("dve", ue[c][:], psu[c][:], bgu[:, e, 8 + j:9 + j], 7.0, ALU.add, ALU.min,
                         R=[psu[c], bgu], W=[ue[c]])
                    T.ts("pool", ue[c][:], ue[c][:], -7.0, 1.0, ALU.max, ALU.add, R=[ue[c]], W=[ue[c]])
                    T.tt("pool", wv_[c][:], gc[c][:], sg[c][:], ALU.mult, R=[gc[c], sg[c]], W=[wv_[c]])
                    T.tt("pool", actT[:, j, tsl], ue[c][:], wv_[c][:], ALU.mult, R=[ue[c], wv_[c]], W=[actT])
            for sub in range(NS):
                for half in range(2):
                    py = psy[ycnt % 2]
                    ycnt += 1
                    for kk in range(8):
                        T.mm(py[:], actT[:, kk, sub * 128:(sub + 1) * 128], Wd_[:, kk, half * 512:(half + 1) * 512],
                             start=(kk == 0), stop=(kk == 7), R=[actT, Wd_], W=[py])
                    a_ = acc[:, sub, half * 512:(half + 1) * 512]
                    T.stt(a_, py[:], gts[tp][:, sub, e:e + 1], a_, ALU.mult, ALU.add, R=[py, gts[tp], acc], W=[acc])
            if e == NE - 1:
                for sub in range(NS):
                    i = t * NS + sub
                    q = i % 2
                    T.dma("sp", xq[q], xt[q][:], x_src[i * 128:(i + 1) * 128, :], R=[b_src], W=[xt[q]])
                    T.tt("dve", acc[:, sub, :], acc[:, sub, :], gf[:], ALU.mult, R=[acc, gf], W=[acc])
                    T.tt("pool", xo[q][:], acc[:, sub, :], xt[q][:], ALU.add, R=[acc, xt[q]], W=[xo[q]])
                    if final:
                        rstd_of(k, xo[q], junk, ss[q], rstd[q])
                        T.stt(xo[q][:], xo[q][:], rstd[q][:, 0:1], fg[:], ALU.mult, ALU.mult,
                              R=[xo[q], rstd[q], fg], W=[xo[q]])
                    T.dma("sp", xoq[q], x_dst[i * 128:(i + 1) * 128, :], xo[q][:], R=[xo[q]], W=[b_dst])
    T.barrier()


def phase_l1_proj(k, hT, hkT, Gtm, Gref):
    nc, T = k.nc, k.T
    with ExitStack() as es:
        W1 = sb(k, es, "W1", [128, 8, D], BF16)
        wf = sb(k, es, "wf", [128, 8, 16], BF16)
        wsl = T.swslot()
        wfs = T.swslot()
        bfb = sb(k, es, "bfb", [128, 16], F32)
        T.dma("sp", T.slot(), bfb[:], bcast_row(k.d["b_f"].rearrange("(o f) -> o f", o=1)), W=[bfb])
        kvf = k.d["w_kvf"].rearrange("(kk p) n -> p kk n", p=128)
        T.dma("pool", wfs, wf[:], kvf[:, :, 2048:2064], W=[wf])
        stg = [sb(k, es, "stg", [128, S], BF16) for _ in range(2)]
        stq = [T.slot() for _ in range(2)]
        vst = [sb(k, es, "vst", [128, D], BF16) for _ in range(2)]
        vsq = [T.slot() for _ in range(2)]
        pp = [ps(k, es, "pp", [128, 512], F32) for _ in range(4)]
        pz = ps(k, es, "pz", [128, 512], F32)
        it = 0
        sidx = 0
        for name, wsrc, src in (("QT", k.d["b_w_q"].rearrange("(kk p) n -> p kk n", p=128), hT),
                                ("KT", kvf[:, :, 0:1024], hkT)):
            T.dma("pool", wsl, W1[:], wsrc, W=[W1])
            for hp in range(8):
                st = stg[sidx % 2]
                for tt_ in range(8):
                    p_ = pp[it % 4]
                    it += 1
                    for kk in range(8):
                        T.mm(p_[:], W1[:, kk, hp * 128:(hp + 1) * 128], src[:, kk, tt_ * 512:(tt_ + 1) * 512],
                             start=(kk == 0), stop=(kk == 7), R=[W1, src], W=[p_])
                    T.cp("act", st[:, tt_ * 512:(tt_ + 1) * 512], p_[:], R=[p_], W=[st])
                T.dma("sp", stq[sidx % 2], k.d[name][hp * 128:(hp + 1) * 128, :], st[:], R=[st], W=[k.db[name]])
                sidx += 1
        T.dma("pool", wsl, W1[:], kvf[:, :, 1024:2048], W=[W1])
        v2 = k.d["V2"].rearrange("h p j c -> p h j c")
        for i in range(NT):
            vs_ = vst[i % 2]
            for half in range(2):
                p_ = pp[it % 4]
                it += 1
                for kk in range(8):
                    T.mm(p_[:], hkT[:, kk, i * 128:(i + 1) * 128], W1[:, kk, half * 512:(half + 1) * 512],
                         start=(kk == 0), stop=(kk == 7), R=[hkT, W1], W=[p_])
                T.cp("act", vs_[:, half * 512:(half + 1) * 512], p_[:], R=[p_], W=[vs_])
            T.dma("sp", vsq[i % 2], v2[:, :, i, :], vs_[:].rearrange("p (h c) -> p h c", h=8), R=[vs_], W=[k.db["V2"]])
        y = sb(k, es, "y", [128, 16], F32)
        lf = sb(k, es, "lf", [128, 16], F32)
        carry = [sb(k, es, "carry", [128, 16], F32) for _ in range(2)]
        T.memset("dve", carry[0][:], 0.0, W=[carry[0]])
        for i in range(NT):
            for kk in range(8):
                T.mm(pz[:, 0:16], hkT[:, kk, i * 128:(i + 1) * 128], wf[:, kk, :], start=(kk == 0), stop=(kk == 7),
                     R=[hkT, wf], W=[pz])
            T.tt("dve", y[:], pz[:, 0:16], bfb[:], ALU.add, R=[pz, bfb], W=[y])
            T.act(y[:], y[:], AF.Exp, scale=-1.0, R=[y], W=[y])
            T.act(lf[:], y[:], AF.Ln, bias=1.0, R=[y], W=[lf])
            T.mm(pz[:, 16:32], k.tri_f[:], lf[:], R=[k.tri_f, lf], W=[pz])
            T.mm(pz[:, 32:48], k.ones_f[:], lf[:], R=[k.ones_f, lf], W=[pz])
            c0, c1 = carry[i % 2], carry[(i + 1) % 2]
            T.tt("dve", Gtm[:, i, :], pz[:, 16:32], c0[:], ALU.add, R=[pz, c0], W=[Gtm])
            T.tt("dve", c1[:], pz[:, 32:48], c0[:], ALU.add, R=[pz, c0], W=[c1])
            T.mm(pz[:, 48:64], k.sel64_f[:], Gtm[:, i, :], R=[k.sel64_f, Gtm], W=[pz])
            T.cp("dve", Gref[:, i, :], pz[:, 48:64], R=[pz], W=[Gref])
    T.barrier()


def phase_fox(k, Gtm, Gref):
    nc, T = k.nc, k.T
    with ExitStack() as es:
        QT = [sb(k, es, "fQT", [128, S], BF16) for _ in range(2)]
        KT = [sb(k, es, "fKT", [128, S], BF16) for _ in range(2)]
        Vs = [sb(k, es, "fVs", [128, 32, 128], BF16) for _ in range(2)]
        lq = [[T.slot() for _ in range(3)] for _ in range(2)]
        VA0 = sb(k, es, "fVA0", [128, 32, 128], BF16)
        V0B = sb(k, es, "fV0B", [128, 32, 128], BF16)
        T.memset("pool", VA0[:], 0.0, W=[VA0])
        T.memset("pool", V0B[:], 0.0, W=[V0B])
        Dcol = [sb(k, es, "Dcol", [128, 2, 4, 32], F32) for _ in range(2)]
        pT = [sb(k, es, "fpT", [128, 2, 512], BF16) for _ in range(3)]
        pT_b = {id(t_): [[Buf("pTr") for _ in range(4)] for _ in range(2)] for t_ in pT}
        rd = sb(k, es, "frd", [128, 512], F32)
        ob = [sb(k, es, "fob", [128, 512], BF16) for _ in range(2)]
        oq = [T.slot() for _ in range(2)]
        pS = [[ps(k, es, "fpS", [128, 512], F32) for _ in range(2)] for _ in range(2)]
        po = ps(k, es, "fpo", [128, 512], F32)
        pd = ps(k, es, "fpd", [128, 512], F32)
        pfill = ps(k, es, "fpfill", [128, 512], F32)
        NFILL = int(os.environ.get("FOX_FILL", "0"))
        v2 = k.d["V2"]

        def load(hp):
            p = hp % 2
            T.dma("sp", lq[p][0], QT[p][:], k.d["QT"][hp * 128:(hp + 1) * 128, :], R=[k.db["QT"]], W=[QT[p]])
            T.dma("sp", lq[p][1], KT[p][:], k.d["KT"][hp * 128:(hp + 1) * 128, :], R=[k.db["KT"]], W=[KT[p]])
            T.dma("sp", lq[p][2], Vs[p][:], v2[hp], R=[k.db["V2"]], W=[Vs[p]])

        load(0)
        jc = 0
        oc = 0
        for hp in range(8):
            p = hp % 2
            if hp + 1 < 8:
                load(hp + 1)
            T.cp("pool", VA0[:, :, 0:64], Vs[p][:, :, 0:64], R=[Vs[p]], W=[VA0])
            T.cp("pool", V0B[:, :, 64:128], Vs[p][:, :, 64:128], R=[Vs[p]], W=[V0B])
            for J in range(8):
                Dc = Dcol[J % 2]
                for hh in range(2):
                    h_ = hp * 2 + hh
                    for a2 in range(4):
                        a = 4 * J + a2
                        T.ts("dve", Dc[:, hh, a2, 0:a + 1], Gtm[:, 0:a + 1, h_], Gref[:, a, h_:h_ + 1], None,
                             ALU.subtract, R=[Gtm, Gref], W=[Dc])
                nj = 4 * J + 4
                st_ = {}

                def s_part(j):
                    nonlocal jc
                    a2min = max(0, j - 4 * J)
                    qs = slice(J * 512 + a2min * 128, (J + 1) * 512)
                    cs = slice(a2min * 128, 512)
                    bufs = pS[jc % 2]
                    pTb = pT[jc % 3]
                    jc += 1
                    st_[j] = (a2min, cs, pTb)
                    for hh in range(2):
                        pb = slice(hh * 64, hh * 64 + 64)
                        T.mm(bufs[hh][:, cs], KT[p][pb, j * 128:(j + 1) * 128], QT[p][pb, qs],
                             R=[KT[p], QT[p]], W=[bufs[hh]])
                    for hh in range(2):
                        for a2 in range(a2min, 4):
                            blk = slice(a2 * 128, (a2 + 1) * 128)
                            rb_ = pT_b[id(pTb)][hh][a2]
                            T.act(pTb[:, hh, blk], bufs[hh][:, blk], AF.Exp, bias=Dc[:, hh, a2, j:j + 1], scale=0.125,
                                  R=[bufs[hh], Dc], W=[rb_], same=False)
                            if j == 4 * J + a2:
                                T.tt("pool", pTb[:, hh, blk], pTb[:, hh, blk], k.mask01[:], ALU.mult,
                                     R=[rb_, k.mask01], W=[rb_])

                def pv_part(j):
                    a2min, cs, pTb = st_[j]
                    for _f in range(NFILL):
                        T.mm(pfill[:], k.onesA0[:], k.mask01w[:], R=[k.onesA0, k.mask01w], W=[pfill])
                    for hh in range(2):
                        T.mm(po[:, cs], (VA0 if hh == 0 else V0B)[:, j, :], pTb[:, hh, cs],
                             start=(j == 0 and hh == 0), stop=(j == nj - 1 and hh == 1),
                             R=[VA0, V0B] + pT_b[id(pTb)][hh][a2min:], W=[po])
                    for hh in range(2):
                        T.mm(pd[:, cs], (k.onesA0 if hh == 0 else k.ones0B)[:], pTb[:, hh, cs],
                             start=(j == 0 and hh == 0), stop=(j == nj - 1 and hh == 1),
                             R=[k.onesA0, k.ones0B] + pT_b[id(pTb)][hh][a2min:], W=[pd])

                s_part(0)
                for j in range(nj):
                    if j + 1 < nj:
                        s_part(j + 1)
                    pv_part(j)
                T.recip(rd[:], pd[:], R=[pd], W=[rd])
                o_ = ob[oc % 2]
                T.tt("dve", o_[:], po[:], rd[:], ALU.mult, R=[po, rd], W=[o_])
                T.dma("sp", oq[oc % 2], k.d["oT"][hp * 128:(hp + 1) * 128, J * 512:(J + 1) * 512], o_[:],
                      R=[o_], W=[k.b_oT])
                oc += 1
    T.barrier()


def phase_moe_sparse(k, layer, xs, xd, final=False):
    nc, T = k.nc, k.T
    x_src, x_dst, b_src, b_dst = k.d[xs], k.d[xd], k.db[xs], k.db[xd]
    NST = BLK // 128
    with ExitStack() as es:
        wgu = [sb(k, es, "wgu", [128, 8, 2 * D], BF16) for _ in range(2)]
        wd = [sb(k, es, "wd", [128, 8, D], BF16) for _ in range(2)]
        wgu_b = [[Buf("wgu%d_%d" % (p_, kk)) for kk in range(8)] for p_ in range(2)]
        wd_b = [[Buf("wd%d_%d" % (p_, kk)) for kk in range(8)] for p_ in range(2)]
        wq8 = [[T.swslot() for _ in range(8)] for _ in range(2)]
        wq8d = [[T.swslot() for _ in range(8)] for _ in range(2)]
        bgu = [sb(k, es, "bgu", [128, 16], F32) for _ in range(2)]
        bdb = [sb(k, es, "bdb", [128, D], F32) for _ in range(2)]
        bq = [T.swslot() for _ in range(2)]
        xr = [sb(k, es, "xr", [128, NST, D], BF16) for _ in range(2)]
        xrq = [T.slot() for _ in range(2)]
        xT = [sb(k, es, "xT", [128, 8, BLK], BF16) for _ in range(2)]
        actT = sb(k, es, "actT", [128, 8, BLK], BF16)
        gc = [sb(k, es, "gc", [128, BLK], F32) for _ in range(3)]
        sg = [sb(k, es, "sg", [128, BLK], F32) for _ in range(3)]
        ue = [sb(k, es, "ue", [128, BLK], F32) for _ in range(3)]
        wv_ = [sb(k, es, "wv", [128, BLK], F32) for _ in range(3)]
        yrow = [sb(k, es, "yrow", [128, D], F32) for _ in range(2)]
        yq = [T.slot() for _ in range(2)]
        psg = [ps(k, es, "psg", [128, 512], F32) for _ in range(3)]
        psu = [ps(k, es, "psu", [128, 512], F32) for _ in range(2)]
        psy = [ps(k, es, "psy", [128, 512], F32) for _ in range(2)]
        ptr = ps(k, es, "ptrm", [128, D], BF16)
        wgu_rows = k.d["w_gu"].rearrange("l e r n -> (l e r) n")
        wd_rows = k.d["w_down"].rearrange("l e r n -> (l e r) n")
        bgu_rows = k.d["b_gu_col"].rearrange("l e p j -> (l e p) j")
        bd_rows = k.d["b_down"].rearrange("l e f -> (l e) f")
        xbv = k.d["xbuf"].rearrange("(b s p) f -> b p s f", p=128, s=NST)
        ybv = k.d["ybuf"].rearrange("(b s p) f -> b s p f", p=128, s=NST)

        def off(ap_):
            return bass.IndirectOffsetOnAxis(ap=ap_, axis=0)

        def load(b):
            p = b % 2
            for kk in range(8):
                T.idma(wq8[p][kk], wgu[p][:, kk, :], None, wgu_rows, off(k.widx[:, b, kk:kk + 1]), 0,
                       R=[k.widx], W=[wgu_b[p][kk]])
            for kk in range(8):
                T.idma(wq8d[p][kk], wd[p][:, kk, :], None, wd_rows, off(k.widx[:, b, kk:kk + 1]), 0,
                       R=[k.widx], W=[wd_b[p][kk]])
            T.idma(bq[p], bgu[p][:], None, bgu_rows, off(k.bidx[:, b, 0:1]), 0, R=[k.bidx], W=[bgu[p]])
            T.idma(bq[p], bdb[p][:], None, bd_rows, off(k.bidx[:, b, 1:2]), 0, R=[k.bidx], W=[bdb[p]])
            T.dma("sp", xrq[p], xr[p][:], xbv[b], R=[k.db["xbuf"]], W=[xr[p]])

        def transposes(b):
            p = b % 2
            for st in range(NST):
                for fj in range(8):
                    T.tr(ptr[:, fj * 128:(fj + 1) * 128], xr[p][:, st, fj * 128:(fj + 1) * 128], k.ident_bf[:],
                         R=[xr[p], k.ident_bf], W=[ptr])
                T.cp("act", xT[p][:, :, st * 128:(st + 1) * 128], ptr[:].rearrange("p (j t) -> p j t", j=8),
                     R=[ptr], W=[xT[p]])

        load(0)
        transposes(0)
        cnt = 0
        ycnt = 0
        yc = 0
        for b in range(NBLK):
            p = b % 2
            if b + 1 < NBLK:
                load(b + 1)
            W_, Wd_ = wgu[p], wd[p]
            xT_ = xT[p]
            for j in range(8):
                c = cnt % 3
                c2 = cnt % 2
                cnt += 1
                for kk in range(8):
                    T.mm(psg[c][:, 0:BLK], W_[:, kk, j * 128:(j + 1) * 128], xT_[:, kk, :],
                         start=(kk == 0), stop=(kk == 7), R=[wgu_b[p][kk], xT_], W=[psg[c]])
                for kk in range(8):
                    T.mm(psu[c2][:, 0:BLK], W_[:, kk, D + j * 128:D + (j + 1) * 128], xT_[:, kk, :],
                         start=(kk == 0), stop=(kk == 7), R=[wgu_b[p][kk], xT_], W=[psu[c2]])
                T.ts("dve", gc[c][:], psg[c][:, 0:BLK], bgu[p][:, j:j + 1], 7.0, ALU.add, ALU.min,
                     R=[psg[c], bgu[p]], W=[gc[c]])
                T.act(ue[c][:], psu[c2][:, 0:BLK], AF.Identity, bias=bgu[p][:, 8 + j:9 + j], R=[psu[c2], bgu[p]], W=[ue[c]])
                T.act(sg[c][:], gc[c][:], AF.Sigmoid, scale=1.702, R=[gc[c]], W=[sg[c]])
                T.ts("dve", ue[c][:], ue[c][:], -7.0, 7.0, ALU.max, ALU.min, R=[ue[c]], W=[ue[c]])
                T.tt("dve", wv_[c][:], gc[c][:], sg[c][:], ALU.mult, R=[gc[c], sg[c]], W=[wv_[c]])
                T.stt(actT[:, j, :], ue[c][:], 1.0, wv_[c][:], ALU.add, ALU.mult, R=[ue[c], wv_[c]], W=[actT])
            if b + 1 < NBLK:
                transposes(b + 1)
            for st in range(NST):
                yr = yrow[yc % 2]
                for half in range(2):
                    py = psy[ycnt % 2]
                    ycnt += 1
                    for kk in range(8):
                        T.mm(py[:], actT[:, kk, st * 128:(st + 1) * 128], Wd_[:, kk, half * 512:(half + 1) * 512],
                             start=(kk == 0), stop=(kk == 7), R=[actT, wd_b[p][kk]], W=[py])
                    T.tt("dve", yr[:, half * 512:(half + 1) * 512], py[:], bdb[p][:, half * 512:(half + 1) * 512],
                         ALU.add, R=[py, bdb[p]], W=[yr])
                T.dma("sp", yq[yc % 2], ybv[b, st], yr[:], R=[yr], W=[k.db["ybuf"]])
                yc += 1
    T.barrier()
    with ExitStack() as es:
        gf = sb(k, es, "gf", [128, D], F32)
        T.dma("sp", T.slot(), gf[:], bcast_row(ada_row(k, layer, ROW_GATE_F)), R=[k.b_ada], W=[gf])
        fg = None
        if final:
            fg = sb(k, es, "fg", [128, D], F32)
            T.dma("sp", T.slot(), fg[:], bcast_row(k.d["final_norm_g"].rearrange("(o f) -> o f", o=1)), W=[fg])
        yg = [[sb(k, es, "yg", [128, D], F32) for _ in range(4)] for _ in range(2)]
        ygq = [[T.swslot() for _ in range(4)] for _ in range(2)]
        xt = [sb(k, es, "xtm", [128, D], F32) for _ in range(2)]
        xq = [T.slot() for _ in range(2)]
        acc = sb(k, es, "cacc", [128, D], F32)
        xo = [sb(k, es, "xo", [128, D], F32) for _ in range(2)]
        xoq = [T.slot() for _ in range(2)]
        junk = sb(k, es, "junk", [128, D], F32)
        ss = [sb(k, es, "ss", [128, 4], F32) for _ in range(2)]
        rstd = [sb(k, es, "rstd", [128, 1], F32) for _ in range(2)]

        def gather(i):
            q = i % 2
            T.dma("sp", xq[q], xt[q][:], x_src[i * 128:(i + 1) * 128, :], R=[b_src], W=[xt[q]])
            for kq in range(4):
                T.idma(ygq[q][kq], yg[q][kq][:], None, k.d["ybuf"],
                       bass.IndirectOffsetOnAxis(ap=k.gidx_all[:, i, kq:kq + 1], axis=0), NROWS - 1,
                       R=[k.gidx_all, k.db["ybuf"]], W=[yg[q][kq]])

        gather(0)
        for i in range(NT):
            q = i % 2
            if i + 1 < NT:
                gather(i + 1)
            T.ts("dve", acc[:], yg[q][0][:], k.gsel_all[:, i, 0:1], None, ALU.mult, R=[yg[q][0], k.gsel_all], W=[acc])
            for kq in range(1, 4):
                T.stt(acc[:], yg[q][kq][:], k.gsel_all[:, i, kq:kq + 1], acc[:], ALU.mult, ALU.add,
                      R=[yg[q][kq], k.gsel_all, acc], W=[acc])
            T.tt("dve", acc[:], acc[:], gf[:], ALU.mult, R=[acc, gf], W=[acc])
            T.tt("pool", xo[q][:], acc[:], xt[q][:], ALU.add, R=[acc, xt[q]], W=[xo[q]])
            if final:
                rstd_of(k, xo[q], junk, ss[q], rstd[q])
                T.stt(xo[q][:], xo[q][:], rstd[q][:, 0:1], fg[:], ALU.mult, ALU.mult, R=[xo[q], rstd[q], fg], W=[xo[q]])
            T.dma("sp", xoq[q], x_dst[i * 128:(i + 1) * 128, :], xo[q][:], R=[xo[q]], W=[b_dst])
    T.barrier()

def build(debug=False, upto=99):
    nc = bass.Bass("TRN2", target_bir_lowering=False)
    k = K()
    k.nc = nc
    k.uid = 0
    k.d = {}
    scr_kind = "ExternalOutput" if debug else "Internal"

    def din(name, shape, dt=F32):
        k.d[name] = nc.dram_tensor(name, list(shape), dt, kind="ExternalInput").ap()

    def dscr(name, shape, dt=F32):
        k.d[name] = nc.dram_tensor(name, list(shape), dt, kind=scr_kind).ap()

    din("x", [S, D])
    din("c_col", [128, 8])
    din("ada_w", [2, D, 6 * D])
    din("ada_b", [2, 6 * D])
    din("norm_mix_g", [2, D])
    din("norm_ffn_g", [2, D])
    din("a_w_qkv", [D, 9216])
    din("a_w_o", [D, D])
    din("dil_bias", [3, 8, 128, 512])
    din("dil_mask", [128, 512])
    din("kv_norm_g", [D])
    din("w_kvf", [D, 2064])
    din("b_f", [16])
    din("b_w_q", [D, D])
    din("b_w_o", [D, D])
    din("tri_f", [128, 128])
    din("ones_f", [128, 128])
    din("sel64_f", [128, 128])
    din("tris_f", [128, 128])
    din("bstart", [128, NBLK])
    din("wbase", [128, 8])
    din("mask01", [128, 128], BF16)
    din("router_w", [2, D, NE])
    din("router_b", [2, NE])
    din("w_gu", [2, NE, D, 2 * D])
    din("b_gu_col", [2, NE, 128, 16])
    din("w_down", [2, NE, D, D])
    din("b_down", [2, NE, D])
    din("final_norm_g", [D])
    din("ident_f", [128, 128])
    din("ident_bf", [128, 128], BF16)
    din("onesA0", [128, 128], BF16)
    din("ones0B", [128, 128], BF16)
    dscr("ada_scr", [12, D])
    dscr("oT", [D, S], BF16)
    dscr("h2T", [D, S], BF16)
    dscr("gates", [S, NE])
    dscr("gatesT", [NE, S])
    dscr("QT", [D, S], BF16)
    dscr("KT", [D, S], BF16)
    dscr("V2", [8, 128, 32, 128], BF16)
    dscr("xc", [S, D])
    dscr("h2tm", [S, D], BF16)
    dscr("xbuf", [NROWS, D], BF16)
    dscr("ybuf", [NROWS, D])
    dscr("xa", [S, D])
    dscr("xb", [S, D])
    k.d["out"] = nc.dram_tensor("out", [S, D], F32, kind="ExternalOutput").ap()
    k.b_x = Buf("x")
    k.b_ada = Buf("ada_scr")
    k.b_oT = Buf("oT")
    k.db = {n: Buf(n) for n in ("x", "xa", "xb", "xc", "out", "QT", "KT", "V2", "xbuf", "ybuf", "h2tm")}
    k.b_h2T = Buf("h2T")
    k.b_gates = Buf("gates")

    with ExitStack() as es:
        T = Tr(nc, es)
        k.T = T
        T.bound_reg = nc.gpsimd.to_reg(2 * NE * 1024 - 1)
        k.ident_bf = sb(k, es, "ident_bf", [128, 128], BF16)
        k.onesA0 = sb(k, es, "onesA0", [128, 128], BF16)
        k.ones0B = sb(k, es, "ones0B", [128, 128], BF16)
        k.dmask = sb(k, es, "dmask", [128, 512], F32)
        k.ident_f = sb(k, es, "ident_f", [128, 128], F32)
        q0 = T.slot()
        T.dma("sp", q0, k.ident_f[:], k.d["ident_f"], W=[k.ident_f])
        k.bstart = sb(k, es, "bstart", [128, NBLK], F32)
        T.dma("sp", q0, k.bstart[:], k.d["bstart"], W=[k.bstart])
        k.wbase = sb(k, es, "wbase", [128, 8], F32)
        T.dma("sp", q0, k.wbase[:], k.d["wbase"], W=[k.wbase])
        k.widx = sb(k, es, "widx", [128, NBLK, 8], I32)
        k.bidx = sb(k, es, "bidx", [128, NBLK, 2], I32)
        k.mask01w = sb(k, es, "mask01w", [128, 512], BF16)
        T.memset("pool", k.mask01w[:], 1.0, W=[k.mask01w])
        k.gidx_all = sb(k, es, "gidx_all", [128, NT, 4], I32)
        k.gsel_all = sb(k, es, "gsel_all", [128, NT, 4], F32)
        for nm, dt_ in (("tri_f", F32), ("ones_f", F32), ("sel64_f", F32), ("tris_f", F32), ("mask01", BF16)):
            t_ = sb(k, es, nm, [128, 128], dt_)
            setattr(k, nm, t_)
            T.dma("sp", q0, t_[:], k.d[nm], W=[t_])
        T.dma("sp", q0, k.ident_bf[:], k.d["ident_bf"], W=[k.ident_bf])
        T.dma("sp", q0, k.onesA0[:], k.d["onesA0"], W=[k.onesA0])
        T.dma("sp", q0, k.ones0B[:], k.d["ones0B"], W=[k.ones0B])
        T.dma("sp", q0, k.dmask[:], k.d["dil_mask"], W=[k.dmask])

        if upto >= 0:
            phase_ada(k)
        with ExitStack() as es1:
            if upto >= 1:
                hT, _ = phase_prenorm(k, es1, "x", 0)
            if upto >= 2:
                phase_dilated(k, hT)
        if upto >= 3:
            phase_post_attn(k, 0, k.d["a_w_o"], "x", "xa")
        if upto >= 4:
            phase_moe_sparse(k, 0, "xa", "xb")
        if upto >= 5:
            with ExitStack() as es2:
                Gtm = sb(k, es2, "Gtm", [128, NT, 16], F32)
                Gref = sb(k, es2, "Gref", [128, NT, 16], F32)
                with ExitStack() as es1:
                    hT, hkT = phase_prenorm(k, es1, "xb", 1, with_kv=True)
                    phase_l1_proj(k, hT, hkT, Gtm, Gref)
                if upto >= 6:
                    phase_fox(k, Gtm, Gref)
        if upto >= 7:
            phase_post_attn(k, 1, k.d["b_w_o"], "xb", "xc")
        if upto >= 8:
            phase_moe_sparse(k, 1, "xc", "out", final=True)
        T.barrier()
        T.barrier()
    return nc


def t5_bucket_np(n):
    max_exact = 16
    nf = np.maximum(n, 1).astype(np.float32)
    large = max_exact + (np.log(nf / np.float32(max_exact)) / np.float32(math.log(2048 / max_exact))
                         * np.float32(32 - max_exact)).astype(np.int32)
    large = np.minimum(large, 31)
    return np.where(n < max_exact, n, large)


def host_consts(rel_bias):
    kk = np.arange(128)[:, None]
    qq = np.arange(128)[None, :]
    bias = np.zeros((3, 8, 128, 2, 2, 128), np.float32)
    mask = np.zeros((128, 2, 2, 128), np.float32)
    for kc in range(2):
        delta = qq - kk + 128 * (1 - kc)
        valid = (delta >= 0) & (delta <= 128)
        mask[:, :, kc, :] = np.where(valid, 0.0, NEG)[:, None, :]
        for g, (_, d) in enumerate(DIL):
            bucket = t5_bucket_np(np.clip(delta, 0, None) * d)
            for hp in range(8):
                for hh in range(2):
                    bias[g, hp, :, hh, kc, :] = rel_bias[bucket, g * 16 + hp * 2 + hh]
    ident = np.eye(128, dtype=np.float32).astype(ml_dtypes.bfloat16)
    onesA0 = np.zeros((128, 128), np.float32)
    onesA0[:, :64] = 1
    ones0B = np.zeros((128, 128), np.float32)
    ones0B[:, 64:] = 1
    return dict(dil_bias=bias.reshape(3, 8, 128, 512), dil_mask=mask.reshape(128, 512), ident_bf=ident,
                ident_f=np.eye(128, dtype=np.float32),
                tri_f=np.triu(np.ones((128, 128), np.float32)),
                ones_f=np.ones((128, 128), np.float32),
                tris_f=np.triu(np.ones((128, 128), np.float32), 1),
                bstart=np.repeat((np.arange(NBLK, dtype=np.float32) * BLK)[None, :], 128, axis=0),
                wbase=(np.arange(8, dtype=np.float32)[None, :] * 128 + np.arange(128, dtype=np.float32)[:, None]),
                sel64_f=np.repeat((np.arange(128) == 64).astype(np.float32)[:, None], 128, axis=1),
                mask01=np.triu(np.ones((128, 128), np.float32)).astype(ml_dtypes.bfloat16),
                onesA0=onesA0.astype(ml_dtypes.bfloat16), ones0B=ones0B.astype(ml_dtypes.bfloat16))


def make_in_maps(inputs, cores):
    hc = host_consts(np.asarray(inputs["rel_bias"], np.float32))
    shared = dict(b_gu_col=np.ascontiguousarray(
        np.asarray(inputs["b_gu"]).reshape(2, NE, 16, 128).transpose(0, 1, 3, 2)))
    maps = []
    for b in cores:
        m = dict(hc)
        m["x"] = np.ascontiguousarray(inputs["x"][b])
        m["c_col"] = np.ascontiguousarray(inputs["c"][b].reshape(8, 128).T)
        m["ada_w"] = inputs["ada_w"]
        m["ada_b"] = inputs["ada_b"]
        m["norm_mix_g"] = inputs["norm_mix_g"]
        m["norm_ffn_g"] = inputs["norm_ffn_g"]
        m["a_w_qkv"] = inputs["a_w_qkv"][0]
        m["a_w_o"] = inputs["a_w_o"][0]
        m["kv_norm_g"] = inputs["kv_norm_g"]
        m["w_kvf"] = inputs["w_kvf"]
        m["b_f"] = inputs["b_f"]
        m["b_w_q"] = inputs["b_w_q"][0]
        m["b_w_o"] = inputs["b_w_o"][0]
        m["router_w"] = inputs["router_w"]
        m["router_b"] = inputs["router_b"]
        m["w_gu"] = inputs["w_gu"]
        m["w_down"] = inputs["w_down"]
        m["b_down"] = inputs["b_down"]
        m["final_norm_g"] = inputs["final_norm_g"]
        m["b_gu_col"] = shared["b_gu_col"]
        maps.append(m)
    return maps


def kernel(**inputs):
    inputs = {k_: np.asarray(v) for k_, v in inputs.items()}
    nc = build()
    maps = make_in_maps(inputs, list(range(8)))
    res = run_bass_kernel_spmd(nc, maps, core_ids=list(range(8)))
    return np.stack([r["out"] for r in res.results], axis=0)
```
